# Optimizing a Trainium2 kernel written in Bass

```python
import jax, jax.numpy as jnp
from jax import lax
import numpy as np

D_MODEL = 1024
BATCH = 8
SEQ = 4096
DEPTH = 2

N_MIXERS = 2
ALPHA = (2 * DEPTH) ** 0.25
BETA = (8 * DEPTH) ** -0.25
CHUNK = 64
LN_EPS = 1e-5
NEG_BIG = -1e30

GLA_HEADS = 4
GLA_DK = D_MODEL // 2
GLA_DV = D_MODEL
GLA_RANK = 16
GLA_TEMP = 16.0
GLA_IN = 2 * GLA_DK + 2 * GLA_DV + GLA_RANK

MLSTM_HEADS = 4
MLSTM_DH = D_MODEL // MLSTM_HEADS
CONV_W = 4
MLSTM_IN = 4 * D_MODEL + 2 * MLSTM_HEADS

N_EXPERTS = 16
N_GROUPS = 4
TOP_K = 2
D_FF = D_MODEL // 2

N_GLA_LAYERS = (DEPTH + 1) // 2
N_MLSTM_LAYERS = DEPTH // 2

kernel_name = "hybrid_gla_mlstm_grouped_moe_deepnorm"


def layer_norm(x, g, b):
    x32 = x.astype(jnp.float32)
    mu = x32.mean(-1, keepdims=True)
    var = jnp.square(x32 - mu).mean(-1, keepdims=True)
    return (x32 - mu) * lax.rsqrt(var + LN_EPS) * g + b


def head_norm(o, g, n_heads):
    B, S, W = o.shape
    oh = o.astype(jnp.float32).reshape(B, S, n_heads, W // n_heads)
    mu = oh.mean(-1, keepdims=True)
    var = jnp.square(oh - mu).mean(-1, keepdims=True)
    return ((oh - mu) * lax.rsqrt(var + LN_EPS)).reshape(B, S, W) * g


def to_chunks(t, n_heads):
    B, S, _ = t.shape
    t = t.reshape(B, S // CHUNK, CHUNK, n_heads, -1)
    return t.transpose(0, 3, 1, 2, 4)


def from_chunks(t):
    B, H, N, C, d = t.shape
    return t.transpose(0, 2, 3, 1, 4).reshape(B, N * C, H * d)


def gate_chunks(g):
    B, S, H = g.shape
    return g.reshape(B, S // CHUNK, CHUNK, H).transpose(0, 3, 1, 2)


def causal_mask():
    return jnp.tril(jnp.ones((CHUNK, CHUNK), dtype=bool))


def gla_mixer(x, w_in, w_gate_up, b_gate, norm_g, w_out):
    B, S, _ = x.shape
    H = GLA_HEADS
    dk, dv = GLA_DK // H, GLA_DV // H
    proj = x @ w_in
    q, k, v, r, g_lr = jnp.split(
        proj, [GLA_DK, 2 * GLA_DK, 2 * GLA_DK + GLA_DV, 2 * GLA_DK + 2 * GLA_DV], axis=-1)
    log_a = jax.nn.log_sigmoid((g_lr @ w_gate_up + b_gate).astype(jnp.float32)) / GLA_TEMP
    q = to_chunks(q.astype(jnp.float32) * dk ** -0.5, H)
    k = to_chunks(k.astype(jnp.float32), H)
    v = to_chunks(v.astype(jnp.float32), H)
    bcum = jnp.cumsum(to_chunks(log_a, H), axis=3)
    b_last = bcum[:, :, :, -1:, :]
    q_e = q * jnp.exp(bcum)
    scores = jnp.einsum('bhnik,bhnjk->bhnij', q_e, k * jnp.exp(-bcum))
    scores = jnp.where(causal_mask(), scores, 0.0)
    o_intra = jnp.einsum('bhnij,bhnjv->bhniv', scores, v)
    k_dec = k * jnp.exp(b_last - bcum)
    decay = jnp.exp(b_last[:, :, :, 0, :])

    def step(state, inp):
        qe_n, kd_n, v_n, dec_n = inp
        o_n = jnp.einsum('bhik,bhkv->bhiv', qe_n, state)
        state = dec_n[..., None] * state + jnp.einsum('bhjk,bhjv->bhkv', kd_n, v_n)
        return state, o_n

    s0 = jnp.zeros((B, H, dk, dv), jnp.float32)
    mv = lambda t: jnp.moveaxis(t, 2, 0)
    _, o_inter = lax.scan(step, s0, (mv(q_e), mv(k_dec), mv(v), mv(decay)))
    o = from_chunks(o_intra + jnp.moveaxis(o_inter, 0, 2))
    o = head_norm(o, norm_g, H) * jax.nn.silu(r.astype(jnp.float32))
    return o @ w_out


def causal_conv(u, w, bias):
    S = u.shape[1]
    up = jnp.pad(u, ((0, 0), (CONV_W - 1, 0), (0, 0)))
    return sum(up[:, j:j + S] * w[j] for j in range(CONV_W)) + bias


def mlstm_mixer(x, w_in, b_gates, conv_w, conv_b, norm_g, w_out):
    B, S, D = x.shape
    H, dh = MLSTM_HEADS, MLSTM_DH
    proj = x @ w_in
    qk, v, o_pre, gates = jnp.split(proj, [2 * D, 3 * D, 4 * D], axis=-1)
    qk = jax.nn.silu(causal_conv(qk, conv_w, conv_b))
    q, k = jnp.split(qk, 2, axis=-1)
    gates = (gates + b_gates).astype(jnp.float32)
    i_pre = gate_chunks(gates[..., :H])
    log_f = gate_chunks(jax.nn.log_sigmoid(gates[..., H:]))
    q = to_chunks(q.astype(jnp.float32), H)
    k = to_chunks(k.astype(jnp.float32) * dh ** -0.5, H)
    v = to_chunks(v.astype(jnp.float32), H)
    mask = causal_mask()

    def step(carry, inp):
        c_st, n_st, m_st = carry
        q_n, k_n, v_n, i_n, f_n = inp
        bcum = jnp.cumsum(f_n, axis=-1)
        d_mat = bcum[..., :, None] - bcum[..., None, :] + i_n[..., None, :]
        d_mat = jnp.where(mask, d_mat, -jnp.inf)
        inter = bcum + m_st[..., None]
        m_row = jnp.maximum(inter, d_mat.max(-1))
        p = jnp.exp(d_mat - m_row[..., None])
        w_inter = jnp.exp(inter - m_row)
        s = jnp.einsum('bhid,bhjd->bhij', q_n, k_n) * p
        num = w_inter[..., None] * jnp.einsum('bhid,bhde->bhie', q_n, c_st) \
            + jnp.einsum('bhij,bhje->bhie', s, v_n)
        den = w_inter * jnp.einsum('bhid,bhd->bhi', q_n, n_st) + s.sum(-1)
        h = num / jnp.maximum(jnp.abs(den), jnp.exp(-m_row))[..., None]
        b_last = bcum[..., -1]
        logit_k = b_last[..., None] - bcum + i_n
        m_new = jnp.maximum(b_last + m_st, logit_k.max(-1))
        w_k = jnp.exp(logit_k - m_new[..., None])
        w_old = jnp.exp(b_last + m_st - m_new)
        c_st = w_old[..., None, None] * c_st + jnp.einsum('bhj,bhjd,bhje->bhde', w_k, k_n, v_n)
        n_st = w_old[..., None] * n_st + jnp.einsum('bhj,bhjd->bhd', w_k, k_n)
        return (c_st, n_st, m_new), h

    carry0 = (jnp.zeros((B, H, dh, dh), jnp.float32),
              jnp.zeros((B, H, dh), jnp.float32),
              jnp.full((B, H), NEG_BIG, jnp.float32))
    mv = lambda t: jnp.moveaxis(t, 2, 0)
    _, hs = lax.scan(step, carry0, (mv(q), mv(k), mv(v), mv(i_pre), mv(log_f)))
    h = from_chunks(jnp.moveaxis(hs, 0, 2))
    h = head_norm(h, norm_g, H) * jax.nn.sigmoid(o_pre.astype(jnp.float32))
    return h @ w_out


def grouped_moe(x, w_router, w1, w3, w2):
    B, S, D = x.shape
    t = x.reshape(-1, D)
    T = t.shape[0]
    per_group = N_EXPERTS // N_GROUPS
    aff = jax.nn.sigmoid((t @ w_router).astype(jnp.float32))
    group_score = lax.top_k(aff.reshape(T, N_GROUPS, per_group), TOP_K)[0].sum(-1)
    g_sel = jnp.argmax(group_score, axis=-1)
    in_group = (jnp.arange(N_EXPERTS) // per_group)[None, :] == g_sel[:, None]
    top_val, top_idx = lax.top_k(jnp.where(in_group, aff, -1.0), TOP_K)
    gates_k = top_val / top_val.sum(-1, keepdims=True)
    gates = jnp.einsum('tk,tke->te', gates_k, jax.nn.one_hot(top_idx, N_EXPERTS, dtype=jnp.float32))
    y = jnp.zeros((T, D), jnp.float32)
    for e in range(N_EXPERTS):
        h = jax.nn.silu(t @ w1[e]) * (t @ w3[e])
        y = y + gates[:, e:e + 1] * (h @ w2[e])
    return y.reshape(B, S, D)


def setup_inputs(seed: int = 0) -> dict:
    key = jax.random.key(seed)
    ks = jax.random.split(key, 24)
    f32 = jnp.float32
    nrm = lambda k, shape, scale: jax.random.normal(k, shape, f32) * scale
    D, NG, NM = D_MODEL, N_GLA_LAYERS, N_MLSTM_LAYERS
    b_gates = jnp.concatenate(
        [nrm(ks[8], (NM, MLSTM_HEADS), 0.1),
         jnp.linspace(3.0, 6.0, MLSTM_HEADS, dtype=f32)[None, :] + nrm(ks[9], (NM, MLSTM_HEADS), 0.1)],
        axis=-1)
    return {
        "x": nrm(ks[0], (BATCH, SEQ, D), 1.0),
        "gla_w_in": nrm(ks[1], (NG, D, GLA_IN), D ** -0.5),
        "gla_w_gate_up": nrm(ks[2], (NG, GLA_RANK, GLA_DK), GLA_RANK ** -0.5),
        "gla_b_gate": nrm(ks[3], (NG, GLA_DK), 0.1),
        "gla_norm_g": 1.0 + nrm(ks[4], (NG, GLA_DV), 0.02),
        "gla_w_out": nrm(ks[5], (NG, GLA_DV, D), GLA_DV ** -0.5 * BETA),
        "mlstm_w_in": nrm(ks[6], (NM, D, MLSTM_IN), D ** -0.5),
        "mlstm_b_gates": b_gates,
        "mlstm_conv_w": nrm(ks[10], (NM, CONV_W, 2 * D), CONV_W ** -0.5),
        "mlstm_conv_b": nrm(ks[11], (NM, 2 * D), 0.02),
        "mlstm_norm_g": 1.0 + nrm(ks[12], (NM, D), 0.02),
        "mlstm_w_out": nrm(ks[13], (NM, D, D), D ** -0.5 * BETA),
        "router_w": nrm(ks[14], (D, N_EXPERTS), D ** -0.5),
        "moe_w1": nrm(ks[15], (DEPTH, N_EXPERTS, D, D_FF), D ** -0.5),
        "moe_w3": nrm(ks[16], (DEPTH, N_EXPERTS, D, D_FF), D ** -0.5),
        "moe_w2": nrm(ks[17], (DEPTH, N_EXPERTS, D_FF, D), D_FF ** -0.5 * BETA),
        "ln_g": 1.0 + nrm(ks[18], (DEPTH, 2, D), 0.02),
        "ln_b": nrm(ks[19], (DEPTH, 2, D), 0.02),
    }


def reference(x, gla_w_in, gla_w_gate_up, gla_b_gate, gla_norm_g, gla_w_out,
              mlstm_w_in, mlstm_b_gates, mlstm_conv_w, mlstm_conv_b, mlstm_norm_g, mlstm_w_out,
              router_w, moe_w1, moe_w3, moe_w2, ln_g, ln_b):
    h = x
    for i in range(DEPTH):
        j = i // N_MIXERS
        if i % N_MIXERS == 0:
            mix = gla_mixer(h, gla_w_in[j], gla_w_gate_up[j], gla_b_gate[j], gla_norm_g[j], gla_w_out[j])
        else:
            mix = mlstm_mixer(h, mlstm_w_in[j], mlstm_b_gates[j], mlstm_conv_w[j], mlstm_conv_b[j],
                              mlstm_norm_g[j], mlstm_w_out[j])
        h = layer_norm(ALPHA * h + mix, ln_g[i, 0], ln_b[i, 0])
        ffn = grouped_moe(h, router_w, moe_w1[i], moe_w3[i], moe_w2[i])
        h = layer_norm(ALPHA * h + ffn, ln_g[i, 1], ln_b[i, 1])
    return h
```

```python
import numpy as np
import ml_dtypes
from contextlib import ExitStack
import concourse.bass as bass
import concourse.mybir as mybir
from concourse.bass import IndirectOffsetOnAxis
from concourse.bass_utils import run_bass_kernel_spmd

F32 = mybir.dt.float32
BF16 = mybir.dt.bfloat16
AF = mybir.ActivationFunctionType
ALU = mybir.AluOpType
AX = mybir.AxisListType

D = 1024
ALPHA = (2 * 2) ** 0.25
LN_EPS = 1e-5
NE = 16
DFF = 512
import os
HEAD_SKEW = os.environ.get('HEAD_SKEW', '0') == '1'
I32 = mybir.dt.int32
TSL = 1024


class Reg:
    __slots__ = ("name", "w", "r")

    def __init__(self, name=""):
        self.name = name
        self.w = None
        self.r = {}


class Clock:
    def __init__(self, fw, name, step):
        self.name = name
        self.step = step
        self.sem = fw.stack.enter_context(fw.nc.semaphore(name))
        self.count = 0
        self.is_dma = step == 16


class Eng:
    def __init__(self, fw, name, eng, pe=False):
        self.name = name
        self.eng = eng
        self.clock = Clock(fw, "s_" + name, 1)
        self.seen = {}
        self.pe = pe


class FW:
    def __init__(self, nc, stack):
        self.nc = nc
        self.stack = stack
        self.pe = Eng(self, "pe", nc.tensor, pe=True)
        self.act = Eng(self, "act", nc.scalar)
        self.dve = Eng(self, "dve", nc.vector)
        self.pool = Eng(self, "pool", nc.gpsimd)
        self.sp = Eng(self, "sp", nc.sync)
        self.n_inst = 0
        self.n_wait = 0
        self._dmaclk = {}

    def dma_clock(self, name):
        if name not in self._dmaclk:
            self._dmaclk[name] = Clock(self, "d_" + name, 16)
        return self._dmaclk[name]

    def _deps(self, E, reads, writes, skip_clock=None):
        deps = {}

        def add(cv):
            c, v = cv
            if deps.get(c, 0) < v:
                deps[c] = v
        for r in reads:
            if r.w is not None:
                add(r.w)
        for w in writes:
            if w.w is not None and w.w[0] is not skip_clock:
                add(w.w)
            for cv in w.r.items():
                add(cv)
        for c, v in deps.items():
            if c.is_dma:
                v = c.count
            if c is E.clock and E.pe:
                continue
            if E.seen.get(c, 0) >= v:
                continue
            E.eng.wait_ge(c.sem, v)
            E.seen[c] = v
            self.n_wait += 1

    def op(self, E, fn, reads=(), writes=(), sig=True, **kw):
        self._deps(E, reads, writes)
        ins = fn(**kw)
        if sig:
            ins.then_inc(E.clock.sem, 1)
            E.clock.count += 1
            cnt = E.clock.count
        else:
            cnt = E.clock.count + 1
        for r in reads:
            r.r[E.clock] = cnt
        for w in writes:
            w.w = (E.clock, cnt)
            w.r = {}
        self.n_inst += 1
        return ins

    def dma(self, Q, clk, out, in_, reads=(), writes=(), **kw):
        if Q is self.pool and not clk.name.endswith("_sw"):
            clk = self.dma_clock(clk.name[2:] + "_sw")
        self._deps(Q, reads, writes, skip_clock=clk)
        ins = Q.eng.dma_start(out=out, in_=in_, **kw)
        ins.then_inc(clk.sem, 16)
        clk.count += 16
        for r in reads:
            r.r[clk] = clk.count
        for w in writes:
            w.w = (clk, clk.count)
            w.r = {}
        self.n_inst += 1
        return ins

    def wait_all(self, E, clocks):
        for c in clocks:
            if c.count > 0 and E.seen.get(c, 0) < c.count:
                E.eng.wait_ge(c.sem, c.count)
                E.seen[c] = c.count


class Prog:
    def __init__(self, S):
        self.S = S
        self.NB = S // 128
        self.nc = bass.Bass("TRN2", target_bir_lowering=False)
        self.stack = ExitStack()
        self.fw = FW(self.nc, self.stack)
        self.dram = {}
        self._uid = 0

    def din(self, name, shape, dt=F32):
        t = self.nc.dram_tensor(name, list(shape), dt, kind="ExternalInput").ap()
        self.dram[name] = t
        return t

    def dout(self, name, shape, dt=F32):
        t = self.nc.dram_tensor(name, list(shape), dt, kind="ExternalOutput").ap()
        self.dram[name] = t
        return t

    def dscr(self, name, shape, dt=F32):
        t = self.nc.dram_tensor(name, list(shape), dt, kind="Internal").ap()
        self.dram[name] = t
        return t

    def sb(self, name, shape, dt, stack=None):
        self._uid += 1
        return (stack or self.stack).enter_context(self.nc.sbuf_tensor(f"{name}_{self._uid}", list(shape), dt))

    def ps(self, name, shape, dt, stack=None):
        self._uid += 1
        return (stack or self.stack).enter_context(self.nc.psum_tensor(f"{name}_{self._uid}", list(shape), dt))

    def reg(self, name=""):
        return Reg(name)

    def regs(self, n, name=""):
        return [Reg(f"{name}{i}") for i in range(n)]


def setup_common(P):
    nc, fw = P.nc, P.fw
    S, NB = P.S, P.NB
    P.xT = P.sb("xT", [128, 8, S], BF16)
    P.R_xT = P.regs(NB, "xT")
    P.identb = P.sb("identb", [128, 128], BF16)
    P.identf = P.sb("identf", [128, 128], F32)
    P.R_const = P.reg("const")
    c_identb = P.din("c_identb", [128, 128], BF16)
    c_identf = P.din("c_identf", [128, 128], F32)
    clk = fw.dma_clock("const")
    fw.dma(fw.sp, clk, P.identb[:], c_identb, writes=[P.R_const])
    fw.dma(fw.sp, clk, P.identf[:], c_identf, writes=[P.R_const])
    P.mhalf = P.sb("mhalf", [128, 1], F32)
    fw.op(fw.pool, nc.gpsimd.memset, writes=[P.R_const], ap=P.mhalf[:], constant=-0.5)
    P.ln_stats = [P.sb(f"ln_stats{i}", [128, 2, 6], F32) for i in range(2)]
    P.ln_mv = [P.sb(f"ln_mv{i}", [128, 2], F32) for i in range(2)]
    P.ln_rstd = [P.sb(f"ln_rstd{i}", [128, 1], F32) for i in range(2)]
    P.R_ln = [P.reg(f"ln{i}") for i in range(2)]
    _zb = P.sb("zb", [128, D], BF16)
    P.zb = [_zb, _zb]
    _rzb = P.reg("zb")
    P.R_zb = [_rzb, _rzb]
    P.ln_ctr = 0
    P.pTR = P.ps("pTR", [128, 8, 128], BF16)
    P.R_pTR = P.reg("pTR")


def moe_phase(P, layer, src_h, dst_h, make_xT, T):
    nc, fw = P.nc, P.fw
    S = P.S
    NC_T = T // 128
    NT5 = T // 512
    ln_idx = 2 * layer + 1
    w1 = P.dram["moe_w1"]
    w3 = P.dram["moe_w3"]
    w2 = P.dram["moe_w2"]
    with ExitStack() as st:
        yacc = P.sb("yacc", [128, NC_T, D], F32, st)
        R_y = [[P.reg(f"y{c}_{h}") for h in range(2)] for c in range(NC_T)]
        W1 = [P.sb(f"W1_{s}", [128, 8, DFF], BF16, st) for s in range(2)]
        W3 = [P.sb(f"W3_{s}", [128, 8, DFF], BF16, st) for s in range(2)]
        W2 = [P.sb(f"W2_{s}", [128, 4, D], BF16, st) for s in range(2)]
        R_W1, R_W3, R_W2 = P.regs(2, "W1"), P.regs(2, "W3"), P.regs(2, "W2")
        wclk = [[fw.dma_clock(f"w{m}_{s}") for s in range(2)] for m in range(3)]
        wr = P.sb("wr", [128, 8, NE], BF16, st)
        R_wr = P.reg("wr")
        aff = P.sb("aff", [128, NC_T, NE], F32, st)
        t6 = P.sb("t6", [128, NC_T, 4, 6], F32, st)
        gs = P.sb("gs", [128, NC_T, 4], F32, st)
        gmax = P.sb("gmax", [128, NC_T], F32, st)
        gmask = P.sb("gmask", [128, NC_T, 4], F32, st)
        am = P.sb("am", [128, NC_T, NE], F32, st)
        m1 = P.sb("m1", [128, NC_T], F32, st)
        m2 = P.sb("m2", [128, NC_T], F32, st)
        k1 = P.sb("k1", [128, NC_T, NE], F32, st)
        gates = k1
        k2 = P.sb("k2", [128, NC_T, NE], F32, st)
        R_rt = P.reg("router")
        hs = [P.sb(f"hs{i}", [128, 512], F32, st) for i in range(2)]
        R_hs = P.regs(2, "hs")
        hp = [P.sb(f"hp{i}", [128, 4, 512], BF16, st) for i in range(2)]
        R_hp = [[P.reg(f"hp{i}_{f}") for f in range(4)] for i in range(2)]
        pA = [P.ps(f"pA{i}", [128, 512], F32, st) for i in range(2)]
        pB = [P.ps(f"pB{i}", [128, 512], F32, st) for i in range(2)]
        pY = [P.ps(f"pY{i}", [128, 512], F32, st) for i in range(2)]
        pR = P.ps("pR", [128, NC_T, NE], F32, st)
        R_pA, R_pB, R_pY, R_pR = P.regs(2, "pA"), P.regs(2, "pB"), P.regs(2, "pY"), P.reg("pR")
        yclk = fw.dma_clock("yio")
        lng = P.sb("e_lng", [128, D], F32, st)
        lnb = P.sb("e_lnb", [128, D], F32, st)
        R_lnp = P.reg("elnp")
        fw.dma(fw.sp, fw.dma_clock("elnp"), lng[:], P.dram["ln_g"][layer, 1, :].partition_broadcast(128), writes=[R_lnp])
        fw.dma(fw.sp, fw.dma_clock("elnp"), lnb[:], P.dram["ln_b"][layer, 1, :].partition_broadcast(128), writes=[R_lnp])

        fw.dma(fw.pool, fw.dma_clock("wr"), wr[:], P.dram["router_w"].rearrange("(c p) e -> p c e", p=128), writes=[R_wr])

        def load_w(e, s):
            fw.dma(fw.pool, wclk[0][s], W1[s][:], w1[layer, e].rearrange("(c p) f -> p c f", p=128), writes=[R_W1[s]])
            fw.dma(fw.pool, wclk[1][s], W3[s][:], w3[layer, e].rearrange("(c p) f -> p c f", p=128), writes=[R_W3[s]])
            fw.dma(fw.pool, wclk[2][s], W2[s][:], w2[layer, e].rearrange("(c p) d -> p c d", p=128), writes=[R_W2[s]])

        ycnt = [0]

        def H(e, s, st_i, t, hb):
            tok0 = st_i * T + t * 512
            rx = [P.R_xT[(tok0 // 128) + j] for j in range(4)]
            for fc in range(4):
                a, b = pA[fc % 2], pB[fc % 2]
                for dc in range(8):
                    fw.op(fw.pe, nc.tensor.matmul, reads=rx + [R_W1[s]], writes=[R_pA[fc % 2]], out=a[:],
                          lhsT=W1[s][:, dc, fc * 128:(fc + 1) * 128], rhs=P.xT[:, dc, tok0:tok0 + 512], start=(dc == 0), stop=(dc == 7), sig=(dc == 7))
                for dc in range(8):
                    fw.op(fw.pe, nc.tensor.matmul, reads=rx + [R_W3[s]], writes=[R_pB[fc % 2]], out=b[:],
                          lhsT=W3[s][:, dc, fc * 128:(fc + 1) * 128], rhs=P.xT[:, dc, tok0:tok0 + 512], start=(dc == 0), stop=(dc == 7), sig=(dc == 7))
                fw.op(fw.act, nc.scalar.activation, reads=[R_pA[fc % 2]], writes=[R_hs[fc % 2]], out=hs[fc % 2][:], in_=a[:], func=AF.Silu)
                fw.op(fw.dve, nc.vector.tensor_tensor, reads=[R_hs[fc % 2], R_pB[fc % 2]], writes=[R_hp[hb][fc]],
                      out=hp[hb][:, fc, :], in0=hs[fc % 2][:], in1=b[:], op=ALU.mult)

        def Y(e, s, t, hb):
            for tc in range(4):
                c = t * 4 + tc
                for dh in range(2):
                    k = ycnt[0] % 2
                    ycnt[0] += 1
                    for fc in range(4):
                        fw.op(fw.pe, nc.tensor.matmul, reads=[R_hp[hb][fc], R_W2[s]], writes=[R_pY[k]], out=pY[k][:],
                              lhsT=hp[hb][:, fc, tc * 128:(tc + 1) * 128], rhs=W2[s][:, fc, dh * 512:(dh + 1) * 512],
                              start=(fc == 0), stop=(fc == 3), sig=(fc == 3))
                    ysl = yacc[:, c, dh * 512:(dh + 1) * 512]
                    fw.op(fw.dve, nc.vector.scalar_tensor_tensor, reads=[R_pY[k], R_rt, R_y[c][dh]], writes=[R_y[c][dh]],
                          out=ysl, in0=pY[k][:], scalar=gates[:, c, e:e + 1], in1=ysl, op0=ALU.mult, op1=ALU.add)

        for st_i in range(S // T):
            tokS = st_i * T
            load_w(0, 0)
            load_w(1, 1)
            for c in range(NC_T):
                fw.dma(fw.sp, yclk, yacc[:, c, :], src_h[tokS + c * 128: tokS + (c + 1) * 128, :], writes=R_y[c])
            for c in range(NC_T):
                fw.op(fw.act, nc.scalar.mul, reads=R_y[c], writes=R_y[c], out=yacc[:, c, :], in_=yacc[:, c, :], mul=ALPHA)
            for c in range(NC_T):
                blk = tokS // 128 + c
                for dc in range(8):
                    fw.op(fw.pe, nc.tensor.matmul, reads=[P.R_xT[blk], R_wr], writes=[R_pR], out=pR[:, c, :],
                          lhsT=P.xT[:, dc, blk * 128:(blk + 1) * 128], rhs=wr[:, dc, :], start=(dc == 0), stop=(dc == 7), sig=(dc == 7))
            V = nc.vector
            rt = dict(reads=[R_rt], writes=[R_rt])
            fw.op(fw.act, nc.scalar.activation, reads=[R_pR], writes=[R_rt], out=aff[:], in_=pR[:], func=AF.Sigmoid)
            a4 = aff[:].rearrange("p c (g k) -> p c g k", k=4)
            fw.op(fw.dve, V.tensor_tensor, **rt, out=t6[:, :, :, 0:2], in0=a4[:, :, :, 0:2], in1=a4[:, :, :, 2:4], op=ALU.add)
            fw.op(fw.dve, V.tensor_tensor, **rt, out=t6[:, :, :, 2:5], in0=a4[:, :, :, 0:3], in1=a4[:, :, :, 1:4], op=ALU.add)
            fw.op(fw.dve, V.tensor_tensor, **rt, out=t6[:, :, :, 5:6], in0=a4[:, :, :, 0:1], in1=a4[:, :, :, 3:4], op=ALU.add)
            fw.op(fw.dve, V.tensor_reduce, **rt, out=gs[:], in_=t6[:], axis=AX.X, op=ALU.max)
            fw.op(fw.dve, V.tensor_reduce, **rt, out=gmax[:], in_=gs[:], axis=AX.X, op=ALU.max)
            fw.op(fw.dve, V.tensor_tensor, **rt, out=gmask[:], in0=gs[:], in1=gmax[:].unsqueeze(2).to_broadcast([128, NC_T, 4]), op=ALU.is_equal)
            gm_b = gmask[:].unsqueeze(3).to_broadcast([128, NC_T, 4, 4])
            am4 = am[:].rearrange("p c (g k) -> p c g k", k=4)
            fw.op(fw.dve, V.tensor_tensor, **rt, out=am4, in0=a4, in1=gm_b, op=ALU.mult)
            fw.op(fw.dve, V.tensor_tensor, **rt, out=am4, in0=am4, in1=gm_b, op=ALU.add)
            fw.op(fw.dve, V.tensor_scalar_add, **rt, out=am[:], in0=am[:], scalar1=-1.0)
            fw.op(fw.dve, V.tensor_reduce, **rt, out=m1[:], in_=am[:], axis=AX.X, op=ALU.max)
            fw.op(fw.dve, V.tensor_tensor, **rt, out=k1[:], in0=am[:], in1=m1[:].unsqueeze(2).to_broadcast([128, NC_T, NE]), op=ALU.is_equal)
            fw.op(fw.dve, V.scalar_tensor_tensor, **rt, out=am[:], in0=k1[:], scalar=-2.0, in1=am[:], op0=ALU.mult, op1=ALU.add)
            fw.op(fw.dve, V.tensor_reduce, **rt, out=m2[:], in_=am[:], axis=AX.X, op=ALU.max)
            fw.op(fw.dve, V.tensor_tensor, **rt, out=k2[:], in0=am[:], in1=m2[:].unsqueeze(2).to_broadcast([128, NC_T, NE]), op=ALU.is_equal)
            fw.op(fw.dve, V.tensor_tensor, **rt, out=k1[:], in0=k1[:], in1=m1[:].unsqueeze(2).to_broadcast([128, NC_T, NE]), op=ALU.mult)
            fw.op(fw.dve, V.tensor_tensor, **rt, out=k2[:], in0=k2[:], in1=m2[:].unsqueeze(2).to_broadcast([128, NC_T, NE]), op=ALU.mult)
            fw.op(fw.dve, V.tensor_tensor, **rt, out=k1[:], in0=k1[:], in1=k2[:], op=ALU.add)
            fw.op(fw.dve, V.tensor_tensor, **rt, out=m1[:], in0=m1[:], in1=m2[:], op=ALU.add)
            fw.op(fw.dve, V.reciprocal, **rt, out=m1[:], in_=m1[:])
            fw.op(fw.dve, V.tensor_tensor, **rt, out=gates[:], in0=k1[:], in1=m1[:].unsqueeze(2).to_broadcast([128, NC_T, NE]), op=ALU.mult)
            prev = None
            hb = 0
            for e in range(NE):
                s = e % 2
                for t in range(NT5):
                    H(e, s, st_i, t, hb)
                    if prev is not None:
                        Y(*prev)
                        pe_, ps_, pt_, phb_ = prev
                        if pt_ == NT5 - 1 and pe_ + 2 < NE:
                            load_w(pe_ + 2, ps_)
                    prev = (e, s, t, hb)
                    hb ^= 1
            Y(*prev)
            for c in range(NC_T):
                blk = tokS // 128 + c
                z = yacc[:, c, :]
                _ln_two_regs(P, z, R_y[c], lng, lnb, R_lnp)
                fw.dma(fw.sp, yclk, dst_h[tokS + c * 128: tokS + (c + 1) * 128, :], z, reads=R_y[c])
                if make_xT:
                    _to_xT_two(P, z, R_y[c], blk)
        P.phase_barrier()


def _ln_two_regs(P, z, Rs, lng, lnb, R_lnp, on_dve=False, pow_on_pool=False):
    nc, fw = P.nc, P.fw
    k = P.ln_ctr % 2
    P.ln_ctr += 1
    st, mv, rstd, R = P.ln_stats[k], P.ln_mv[k], P.ln_rstd[k], P.R_ln[k]
    for i in range(2):
        fw.op(fw.dve, nc.vector.bn_stats, reads=Rs, writes=[R], out=st[:, i, :], in_=z[:, i * 512:(i + 1) * 512])
    fw.op(fw.dve, nc.vector.bn_aggr, reads=[R], writes=[R], out=mv[:], in_=st[:].rearrange("p a b -> p (a b)"))
    fw.op(fw.dve, nc.vector.tensor_scalar_add, reads=[R], writes=[R], out=rstd[:], in0=mv[:, 1:2], scalar1=LN_EPS)
    if on_dve and not pow_on_pool:
        fw.op(fw.act, nc.scalar.activation, reads=[R], writes=[R], out=rstd[:], in_=rstd[:], func=AF.Sqrt)
        fw.op(fw.dve, nc.vector.reciprocal, reads=[R], writes=[R], out=rstd[:], in_=rstd[:])
    else:
        fw.op(fw.pool, nc.gpsimd.tensor_tensor, reads=[R, P.R_const], writes=[R], out=rstd[:], in0=rstd[:], in1=P.mhalf[:], op=ALU.pow)
    fw.op(fw.dve, nc.vector.tensor_scalar, reads=list(Rs) + [R], writes=Rs, out=z, in0=z, scalar1=mv[:, 0:1], scalar2=rstd[:],
          op0=ALU.subtract, op1=ALU.mult)
    AE, AI = (fw.dve, nc.vector) if on_dve else (fw.pool, nc.gpsimd)
    fw.op(AE, AI.tensor_tensor, reads=list(Rs) + [R_lnp], writes=Rs, out=z, in0=z, in1=lng[:], op=ALU.mult)
    fw.op(AE, AI.tensor_tensor, reads=list(Rs) + [R_lnp], writes=Rs, out=z, in0=z, in1=lnb[:], op=ALU.add)


def _ln_act(P, z, Rz, lng, lnb, R_lnp, T):
    nc, fw = P.nc, P.fw
    k = T["ctr"][0] % 2
    T["ctr"][0] += 1
    junk, R_junk = T["junk"], T["R_junk"]
    s, R = T["s"][k], T["R"][k]
    A_, V_ = nc.scalar, nc.vector
    fw.op(fw.act, A_.activation, reads=[Rz], writes=[R_junk, R], out=junk[:], in_=z, func=AF.Identity, accum_out=s[:, 0:1])
    fw.op(fw.act, A_.activation, reads=[Rz], writes=[R_junk, R], out=junk[:], in_=z, func=AF.Square, accum_out=s[:, 1:2])
    rr = dict(reads=[R], writes=[R])
    fw.op(fw.dve, V_.tensor_scalar_mul, **rr, out=s[:, 2:3], in0=s[:, 0:1], scalar1=1.0 / D)
    fw.op(fw.dve, V_.tensor_tensor, **rr, out=s[:, 3:4], in0=s[:, 2:3], in1=s[:, 2:3], op=ALU.mult)
    fw.op(fw.dve, V_.scalar_tensor_tensor, **rr, out=s[:, 4:5], in0=s[:, 1:2], scalar=1.0 / D, in1=s[:, 3:4], op0=ALU.mult, op1=ALU.subtract)
    fw.op(fw.dve, V_.tensor_scalar_add, **rr, out=s[:, 4:5], in0=s[:, 4:5], scalar1=LN_EPS)
    fw.op(fw.act, A_.activation, **rr, out=s[:, 5:6], in_=s[:, 4:5], func=AF.Sqrt)
    fw.op(fw.dve, V_.reciprocal, **rr, out=s[:, 5:6], in_=s[:, 5:6])
    fw.op(fw.dve, V_.scalar_tensor_tensor, **rr, out=s[:, 6:7], in0=s[:, 2:3], scalar=-1.0, in1=s[:, 5:6], op0=ALU.mult, op1=ALU.mult)
    fw.op(fw.act, A_.activation, reads=[Rz, R], writes=[Rz], out=z, in_=z, func=AF.Identity, scale=s[:, 5:6], bias=s[:, 6:7])
    fw.op(fw.dve, V_.tensor_tensor, reads=[Rz, R_lnp], writes=[Rz], out=z, in0=z, in1=lng[:], op=ALU.mult)
    fw.op(fw.dve, V_.tensor_tensor, reads=[Rz, R_lnp], writes=[Rz], out=z, in0=z, in1=lnb[:], op=ALU.add)


def _to_xT_two(P, z, Rs, blk):
    nc, fw = P.nc, P.fw
    k = blk % 2
    zb, R_zb = P.zb[k], P.R_zb[k]
    fw.op(fw.act, nc.scalar.copy, reads=Rs, writes=[R_zb], out=zb[:], in_=z)
    for c in range(8):
        fw.op(fw.pe, nc.tensor.transpose, reads=[R_zb, P.R_const], writes=[P.R_pTR], out=P.pTR[:, c, :],
              in_=zb[:, c * 128:(c + 1) * 128], identity=P.identb[:])
    fw.op(fw.dve, nc.vector.tensor_copy, reads=[P.R_pTR], writes=[P.R_xT[blk]], out=P.xT[:, :, blk * 128:(blk + 1) * 128], in_=P.pTR[:])


def _phase_barrier(self):
    fw = self.fw
    clocks = [e.clock for e in (fw.pe, fw.act, fw.dve, fw.pool, fw.sp)] + list(fw._dmaclk.values())
    for E in (fw.pe, fw.act, fw.dve, fw.pool, fw.sp):
        fw.wait_all(E, [c for c in clocks if c is not E.clock])


Prog.phase_barrier = _phase_barrier


def residual_ln_out(P, st_tiles, mix_psum, R_mix, x_src, dst_h, blk, ln_idx, lng, lnb, R_lnp, defer_cast=False, stage=0):
    nc, fw = P.nc, P.fw
    k = blk % 2
    z, R_z, clk = st_tiles["z"][k], st_tiles["R_z"][k], st_tiles["zclk"][k]
    if stage in (0, 1):
        fw.dma(fw.sp, clk, z[:], x_src[blk * 128:(blk + 1) * 128, :], writes=[R_z])
        fw.op(fw.dve, nc.vector.scalar_tensor_tensor, reads=[R_z, R_mix], writes=[R_z], out=z[:], in0=z[:], scalar=ALPHA, in1=mix_psum,
              op0=ALU.mult, op1=ALU.add)
    if stage == 1:
        return
    kk = P.ln_ctr % 2
    P.ln_ctr += 1
    st, mv, rstd, R = P.ln_stats[kk], P.ln_mv[kk], P.ln_rstd[kk], P.R_ln[kk]
    for i in range(2):
        fw.op(fw.dve, nc.vector.bn_stats, reads=[R_z], writes=[R], out=st[:, i, :], in_=z[:, i * 512:(i + 1) * 512])
    fw.op(fw.dve, nc.vector.bn_aggr, reads=[R], writes=[R], out=mv[:], in_=st[:].rearrange("p a b -> p (a b)"))
    fw.op(fw.dve, nc.vector.tensor_scalar_add, reads=[R], writes=[R], out=rstd[:], in0=mv[:, 1:2], scalar1=LN_EPS)
    fw.op(fw.pool, nc.gpsimd.tensor_tensor, reads=[R, P.R_const], writes=[R], out=rstd[:], in0=rstd[:], in1=P.mhalf[:], op=ALU.pow)
    fw.op(fw.dve, nc.vector.tensor_scalar, reads=[R_z, R], writes=[R_z], out=z[:], in0=z[:], scalar1=mv[:, 0:1], scalar2=rstd[:],
          op0=ALU.subtract, op1=ALU.mult)
    fw.op(fw.pool, nc.gpsimd.tensor_tensor, reads=[R_z, R_lnp], writes=[R_z], out=z[:], in0=z[:], in1=lng[:], op=ALU.mult)
    fw.op(fw.pool, nc.gpsimd.tensor_tensor, reads=[R_z, R_lnp], writes=[R_z], out=z[:], in0=z[:], in1=lnb[:], op=ALU.add)
    fw.dma(fw.sp, clk, dst_h[blk * 128:(blk + 1) * 128, :], z[:], reads=[R_z])
    if not defer_cast:
        zb_cast(P, z, R_z, blk)


def zb_cast(P, z, R_z, blk):
    P.fw.op(P.fw.act, P.nc.scalar.copy, reads=[R_z], writes=[P.R_zb[blk % 2]], out=P.zb[blk % 2][:], in_=z[:])


def xT_finish(P, blk):
    nc, fw = P.nc, P.fw
    k = blk % 2
    for c in range(8):
        fw.op(fw.pe, nc.tensor.transpose, reads=[P.R_zb[k], P.R_const], writes=[P.R_pTR], out=P.pTR[:, c, :],
              in_=P.zb[k][:, c * 128:(c + 1) * 128], identity=P.identb[:])
    fw.op(fw.dve, nc.vector.tensor_copy, reads=[P.R_pTR], writes=[P.R_xT[blk]], out=P.xT[:, :, blk * 128:(blk + 1) * 128], in_=P.pTR[:])


def zb_double(P, st):
    zb1 = P.sb("zb1", [128, D], BF16, st)
    old = (P.zb, P.R_zb)
    P.zb = [P.zb[0], zb1]
    P.R_zb = [P.R_zb[0], P.reg("zb1")]
    return old


def mm_group(P, out, pairs, reads, writes):
    nc, fw = P.nc, P.fw
    n = len(pairs)
    for i, (l, r) in enumerate(pairs):
        fw.op(fw.pe, nc.tensor.matmul, reads=reads, writes=writes, out=out, lhsT=l, rhs=r, start=(i == 0), stop=(i == n - 1), sig=(i == n - 1))


def gla_phase(P, x_src, dst_h, ln_idx, p0_src=None):
    nc, fw = P.nc, P.fw
    S, NB = P.S, P.NB
    H, DK, DV = 4, 128, 256
    V, A, G_ = nc.vector, nc.scalar, nc.gpsimd
    w_in = P.dram["gla_w_in"]
    with ExitStack() as st:
        sb = lambda n, s, d: P.sb(n, s, d, st)
        Win = sb("gWin", [128, 8, 3088], BF16)
        Wout = sb("gWout", [128, 8, D], BF16)
        Wgu = sb("gWgu", [16, 512], BF16)
        negb = sb("g_negb", [128, 4], F32)
        ngh = sb("g_ngh", [128, D], F32)
        lng = sb("g_lng", [128, D], F32)
        lnb = sb("g_lnb", [128, D], F32)
        tri = sb("g_tri", [128, 128], F32)
        rmask = sb("g_rmask", [128, 512], F32)
        R_w = P.reg("gw")
        R_lnp = P.reg("glnp")
        wclk = fw.dma_clock("gla_w")
        for dc in range(8):
            fw.dma(fw.pool, wclk, Win[:, dc, :], w_in[0, dc * 128:(dc + 1) * 128, :], writes=[R_w])
        fw.dma(fw.pool, wclk, Wout[:], P.dram["gla_w_out"][0].rearrange("(c p) d -> p c d", p=128), writes=[R_w])
        fw.dma(fw.pool, wclk, Wgu[:], P.dram["gla_w_gate_up"][0], writes=[R_w])
        fw.dma(fw.sp, wclk, negb[:], P.dram["l_gla_b_gate"], writes=[R_w])
        fw.dma(fw.sp, wclk, ngh[:], P.dram["gla_norm_g"][0, :].partition_broadcast(128), writes=[R_w])
        fw.dma(fw.sp, wclk, tri[:], P.dram["c_tri"], writes=[R_w])
        fw.dma(fw.sp, wclk, rmask[:], P.dram["c_rmask"], writes=[R_w])
        fw.dma(fw.sp, wclk, lng[:], P.dram["ln_g"][ln_idx // 2, ln_idx % 2, :].partition_broadcast(128), writes=[R_lnp])
        fw.dma(fw.sp, wclk, lnb[:], P.dram["ln_b"][ln_idx // 2, ln_idx % 2, :].partition_broadcast(128), writes=[R_lnp])
        fw.op(fw.act, A.mul, reads=[R_w], writes=[R_w], out=negb[:], in_=negb[:], mul=-1.0)
        fw.op(fw.act, A.mul, reads=[R_w], writes=[R_w], out=ngh[:], in_=ngh[:], mul=0.5)
        S32 = sb("gS32", [128, H, DV], F32)
        S16 = sb("gS16", [128, H, DV], BF16)
        R_S32, R_S16 = P.regs(H, "S32_"), P.regs(H, "S16_")
        for h in range(H):
            fw.op(fw.pool, G_.memset, writes=[R_S32[h]], ap=S32[:, h, :], constant=0.0)
            fw.op(fw.pool, G_.memset, writes=[R_S16[h]], ap=S16[:, h, :], constant=0.0)
        glr = sb("g_glr", [16, 512], BF16); R_glr = P.reg()
        e1 = sb("g_e1", [128, 512], F32); R_e1 = P.reg()
        cs = sb("g_cs", [128, 512], F32); R_cs = P.reg()
        Eb = sb("g_Eb", [128, H, 512], F32); R_Eb = P.regs(H)
        En = sb("g_En", [128, 512], F32); R_En = P.reg()
        qe = sb("g_qe", [128, H, 512], BF16); R_qe = P.regs(H)
        kn32 = sb("g_kn32", [128, H, 512], F32); R_kn32 = P.regs(H)
        kneg = sb("g_kneg", [128, H, 512], BF16); R_kneg = P.regs(H)
        v_sb = sb("g_v", [128, D], BF16); R_v = P.reg()
        th = sb("g_th", [128, D], F32); R_th = P.reg()
        kd = sb("g_kd", [128, 128], BF16); R_kd = P.reg()
        kdT = sb("g_kdT", [128, 128], BF16); R_kdT = P.reg()
        scT = sb("g_scT", [128, 128], BF16); R_scT = P.reg()
        on = sb("g_on", [128, D], F32); R_on = P.reg()
        og = [sb(f"g_og{i}", [128, D], BF16) for i in range(2)]; R_og = P.regs(2)
        ogT = sb("g_ogT", [128, 8, 128], BF16); R_ogT = P.reg()
        hst = sb("g_hst", [128, H, 6], F32)
        hmv = sb("g_hmv", [128, H, 2], F32)
        hrs = sb("g_hrs", [128, H], F32); R_hn = P.reg()
        _z = sb("g_z", [128, D], F32); _Rz = P.reg()
        tiles = {"z": [_z, _z], "R_z": [_Rz, _Rz], "zclk": [fw.dma_clock("gz")] * 2}
        if p0_src is not None:
            p0clk = fw.dma_clock("p0")
            for bb in range(NB):
                fw.dma(fw.sp, p0clk, on[:], p0_src[bb * 128:(bb + 1) * 128, :], writes=[R_on])
                _to_xT_two(P, on[:], [R_on], bb)
        pFM = [P.ps(f"g_pFM{i}", [128, 512], F32, st) for i in range(2)]; R_pFM = P.regs(2)
        pTM = [P.ps(f"g_pTM{i}", [128, D], F32, st) for i in range(2)]; R_pTM = P.regs(2)
        pSC = P.ps("g_pSC", [128, 512], F32, st); R_pSC = P.reg()
        fmc = [0]

        def fm():
            k = fmc[0] % 2
            fmc[0] += 1
            return pFM[k], R_pFM[k]

        def group_stage(g):
            tok0 = g * 512
            rx = [P.R_xT[4 * g + j] for j in range(4)]
            xg = lambda dc: P.xT[:, dc, tok0:tok0 + 512]
            p, Rp = fm()
            mm_group(P, p[0:16, :], [(Win[:, dc, 3072:3088], xg(dc)) for dc in range(8)], rx + [R_w], [Rp])
            fw.op(fw.act, A.copy, reads=[Rp], writes=[R_glr], out=glr[:], in_=p[0:16, :])
            for h in range(H):
                p, Rp = fm()
                mm_group(P, p[:], [(Wgu[0:16, h * 128:(h + 1) * 128], glr[0:16, :])], [R_glr, R_w], [Rp])
                fw.op(fw.act, A.activation, reads=[Rp, R_w], writes=[R_e1], out=e1[:], in_=p[:], func=AF.Exp, scale=-1.0, bias=negb[:, h:h + 1])
                fw.op(fw.act, A.activation, reads=[R_e1], writes=[R_e1], out=e1[:], in_=e1[:], func=AF.Ln, bias=1.0)
                fw.op(fw.dve, V.tensor_tensor_scan, reads=[R_e1, R_w], writes=[R_cs], out=cs[:], data0=rmask[:], data1=e1[:], initial=0.0,
                      op0=ALU.mult, op1=ALU.add)
                fw.op(fw.act, A.activation, reads=[R_cs], writes=[R_Eb[h]], out=Eb[:, h, :], in_=cs[:], func=AF.Exp, scale=-1.0 / 16.0)
                fw.op(fw.act, A.activation, reads=[R_cs], writes=[R_En], out=En[:], in_=cs[:], func=AF.Exp, scale=1.0 / 16.0)
                p, Rp = fm()
                mm_group(P, p[:], [(Win[:, dc, h * 128:(h + 1) * 128], xg(dc)) for dc in range(8)], rx + [R_w], [Rp])
                fw.op(fw.dve, V.scalar_tensor_tensor, reads=[Rp, R_Eb[h]], writes=[R_qe[h]], out=qe[:, h, :], in0=p[:], scalar=DK ** -0.5,
                      in1=Eb[:, h, :], op0=ALU.mult, op1=ALU.mult)
                p, Rp = fm()
                mm_group(P, p[:], [(Win[:, dc, 512 + h * 128:512 + (h + 1) * 128], xg(dc)) for dc in range(8)], rx + [R_w], [Rp])
                fw.op(fw.dve, V.tensor_tensor, reads=[Rp, R_En], writes=[R_kn32[h]], out=kn32[:, h, :], in0=p[:], in1=En[:], op=ALU.mult)
                fw.op(fw.act, A.copy, reads=[R_kn32[h]], writes=[R_kneg[h]], out=kneg[:, h, :], in_=kn32[:, h, :])
        def front(blk):
            if True:
                g, j = divmod(blk, 4)
                if j == 0:
                    group_stage(g)
                tok0 = g * 512
                ogb, R_ogb = og[blk % 2], R_og[blk % 2]
                ts = slice(tok0 + j * 128, tok0 + (j + 1) * 128)
                ls = slice(j * 128, (j + 1) * 128)
                rxb = [P.R_xT[blk], R_w]
                for half in range(2):
                    mm_group(P, pTM[0][:, half * 512:(half + 1) * 512],
                             [(P.xT[:, dc, ts], Win[:, dc, 1024 + half * 512:1024 + (half + 1) * 512]) for dc in range(8)], rxb, [R_pTM[0]])
                fw.op(fw.act, A.copy, reads=[R_pTM[0]], writes=[R_v], out=v_sb[:], in_=pTM[0][:])
                for half in range(2):
                    mm_group(P, pTM[1][:, half * 512:(half + 1) * 512],
                             [(P.xT[:, dc, ts], Win[:, dc, 2048 + half * 512:2048 + (half + 1) * 512]) for dc in range(8)], rxb, [R_pTM[1]])
                fw.op(fw.act, A.activation, reads=[R_pTM[1]], writes=[R_th], out=th[:], in_=pTM[1][:], func=AF.Tanh, scale=0.5)
                fw.op(fw.dve, V.scalar_tensor_tensor, reads=[R_th, R_pTM[1]], writes=[R_th], out=th[:], in0=th[:], scalar=1.0, in1=pTM[1][:],
                      op0=ALU.add, op1=ALU.mult)
                fw.op(fw.pool, G_.tensor_tensor, reads=[R_th, R_w], writes=[R_th], out=th[:], in0=th[:], in1=ngh[:], op=ALU.mult)

        def front_B(blk):
            if True:
                g, j = divmod(blk, 4)
                tok0 = g * 512
                ogb, R_ogb = og[blk % 2], R_og[blk % 2]
                ls = slice(j * 128, (j + 1) * 128)
                for h in range(H):
                    elast = Eb[:, h, j * 128 + 127:j * 128 + 128]
                    hs_ = slice(h * DV, (h + 1) * DV)
                    fw.op(fw.act, A.activation, reads=[R_kn32[h], R_Eb[h]], writes=[R_kd], out=kd[:], in_=kn32[:, h, ls], func=AF.Identity, scale=elast)
                    fw.op(fw.pe, nc.tensor.transpose, reads=[R_kd, P.R_const], writes=[P.R_pTR], out=P.pTR[:, 0, :], in_=kd[:], identity=P.identb[:])
                    fw.op(fw.dve, V.tensor_copy, reads=[P.R_pTR], writes=[R_kdT], out=kdT[:], in_=P.pTR[:, 0, :])
                    mm_group(P, pSC[:, 0:128], [(kneg[:, h, ls], qe[:, h, ls])], [R_kneg[h], R_qe[h]], [R_pSC])
                    fw.op(fw.dve, V.tensor_tensor, reads=[R_pSC, R_w], writes=[R_scT], out=scT[:], in0=pSC[:, 0:128], in1=tri[:], op=ALU.mult)
                    mm_group(P, pTM[0][:, hs_], [(scT[:], v_sb[:, hs_]), (qe[:, h, ls], S16[:, h, :])], [R_scT, R_v, R_qe[h], R_S16[h]], [R_pTM[0]])
                    mm_group(P, pSC[:, 128:384], [(kdT[:], v_sb[:, hs_])], [R_kdT, R_v], [R_pSC])
                    fw.op(fw.dve, V.scalar_tensor_tensor, reads=[R_S32[h], R_Eb[h], R_pSC], writes=[R_S32[h]], out=S32[:, h, :], in0=S32[:, h, :],
                          scalar=elast, in1=pSC[:, 128:384], op0=ALU.mult, op1=ALU.add)
                    fw.op(fw.pool, G_.tensor_copy, reads=[R_S32[h]], writes=[R_S16[h]], out=S16[:, h, :], in_=S32[:, h, :])
                fw.op(fw.act, A.copy, reads=[R_pTM[0]], writes=[R_on], out=on[:], in_=pTM[0][:])
                for h in range(H):
                    fw.op(fw.dve, V.bn_stats, reads=[R_on], writes=[R_hn], out=hst[:, h, :], in_=on[:, h * DV:(h + 1) * DV])
                for h in range(H):
                    fw.op(fw.dve, V.bn_aggr, reads=[R_hn], writes=[R_hn], out=hmv[:, h, :], in_=hst[:, h, :])
                fw.op(fw.dve, V.tensor_scalar_add, reads=[R_hn], writes=[R_hn], out=hrs[:], in0=hmv[:, :, 1], scalar1=LN_EPS)
                fw.op(fw.pool, G_.tensor_tensor, reads=[R_hn, P.R_const], writes=[R_hn], out=hrs[:], in0=hrs[:], in1=P.mhalf[:].to_broadcast([128, H]), op=ALU.pow)
                for h in range(H):
                    fw.op(fw.dve, V.tensor_scalar, reads=[R_on, R_hn], writes=[R_on], out=on[:, h * DV:(h + 1) * DV], in0=on[:, h * DV:(h + 1) * DV],
                          scalar1=hmv[:, h, 0:1], scalar2=hrs[:, h:h + 1], op0=ALU.subtract, op1=ALU.mult)
                fw.op(fw.dve, V.tensor_tensor, reads=[R_on, R_th], writes=[R_ogb], out=ogb[:], in0=on[:], in1=th[:], op=ALU.mult)

        def tailA(blk):
            if True:
                ogb, R_ogb = og[blk % 2], R_og[blk % 2]
                for c in range(8):
                    fw.op(fw.pe, nc.tensor.transpose, reads=[R_ogb, P.R_const], writes=[P.R_pTR], out=P.pTR[:, c, :], in_=ogb[:, c * 128:(c + 1) * 128],
                          identity=P.identb[:])
                fw.op(fw.act, A.copy, reads=[P.R_pTR], writes=[R_ogT], out=ogT[:], in_=P.pTR[:])
                for half in range(2):
                    mm_group(P, pTM[1][:, half * 512:(half + 1) * 512],
                             [(ogT[:, vc, :], Wout[:, vc, half * 512:(half + 1) * 512]) for vc in range(8)], [R_ogT, R_w], [R_pTM[1]])
                residual_ln_out(P, tiles, pTM[1][:], R_pTM[1], x_src, dst_h, blk, ln_idx, lng, lnb, R_lnp, defer_cast=True)

        for b in range(NB + 1):
            if b < NB:
                front(b)
            if 0 <= b - 1 < NB:
                tailA(b - 1)
            if b < NB:
                front_B(b)
            if 0 <= b - 1 < NB:
                zb_cast(P, _z, _Rz, b - 1)
                xT_finish(P, b - 1)
        P.phase_barrier()


def mlstm_phase(P, x_src, dst_h, ln_idx):
    nc, fw = P.nc, P.fw
    S, NB = P.S, P.NB
    H, DH = 4, 256
    GRP = 256
    V, A, G_ = nc.vector, nc.scalar, nc.gpsimd
    w_in = P.dram["mlstm_w_in"]
    a_dram = P.dscr("a_dram", [4, S])
    NEG = -80.0
    with ExitStack() as st:
        sb = lambda n, s, d: P.sb(n, s, d, st)
        cols = sb("m_cols", [128, NB, 16], F32); R_cols = P.reg()
        woldbc = sb("m_woldbc", [128, H, NB], F32); R_wold = P.reg()
        Wg = sb("m_Wg", [128, 8, 8], BF16)
        bg = sb("m_bg", [4, 2], F32)
        nbf = sb("m_nbf", [4, 1], F32)
        tri = sb("m_tri", [128, 128], F32)
        cwh = sb("m_cwh", [128, 16, 4], F32)
        cbh = sb("m_cbh", [128, 16], F32)
        ngc = sb("m_ngc", [128, 8], F32)
        onesb = sb("m_onesb", [128, 1], BF16)
        R_w = P.reg("mw"); R_lnp = P.reg("mlnp")
        wclk = fw.dma_clock("ml_w")
        fw.dma(fw.pool, wclk, Wg[:], w_in[0, :, 4096:4104].rearrange("(c p) f -> p c f", p=128), writes=[R_w])
        for (t_, name) in [(bg, "l_mb"), (tri, "c_trineg"), (cwh, "l_conv_w"), (cbh, "l_conv_b"), (ngc, "l_mnormg")]:
            fw.dma(fw.sp, wclk, t_[:], P.dram[name], writes=[R_w])
        fw.op(fw.pool, G_.memset, writes=[R_w], ap=onesb[:], constant=1.0)
        fw.op(fw.act, A.mul, reads=[R_w], writes=[R_w], out=nbf[:], in_=bg[:, 1:2], mul=-1.0)
        fw.op(fw.act, A.mul, reads=[R_w], writes=[R_w], out=ngc[:], in_=ngc[:], mul=0.5)
        pFM = [P.ps(f"m_pFM{i}", [128, 512], F32, st) for i in range(2)]; R_pFM = P.regs(2)
        pTM = P.ps("m_pTM", [128, D], F32, st); R_pTM = P.reg()
        pSC = P.ps("m_pSC", [128, 512], F32, st); R_pSC = P.reg()
        pSQ = P.ps("m_pSQ", [128, 512], F32, st); R_pSQ = P.reg()
        pCU = P.ps("m_pCU", [128, 2, 256], F32, st); R_pCU = P.reg()
        fmc = [0]

        def fm():
            k = fmc[0] % 2
            fmc[0] += 1
            return pFM[k], R_pFM[k]

        Win = sb("mWin", [128, 8, 4096], BF16)
        R_win = P.reg("mwin")
        for dc in range(8):
            fw.dma(fw.pool, fw.dma_clock("ml_win"), Win[:, dc, :], w_in[0, dc * 128:(dc + 1) * 128, 0:4096], writes=[R_win])
        with ExitStack() as st2:
            T1 = P.sb("m_T1", [4, S], F32, st2); T2 = P.sb("m_T2", [4, S], F32, st2)
            T3 = P.sb("m_T3", [4, S], F32, st2); T4 = P.sb("m_T4", [4, S], F32, st2)
            sel = P.sb("m_sel", [4, H, 128], F32, st2)
            ones4 = P.sb("m_ones4", [4, 128], F32, st2)
            fw.dma(fw.sp, wclk, sel[:], P.dram["c_sel"], writes=[R_w])
            fw.op(fw.pool, G_.memset, writes=[R_w], ap=ones4[:], constant=1.0)
            mnew = P.sb("m_mnew", [4, NB], F32, st2); mprev = P.sb("m_mprev", [4, NB], F32, st2)
            blast = P.sb("m_blast", [4, NB], F32, st2); dlt = P.sb("m_dlt", [4, NB], F32, st2); wold = P.sb("m_wold", [4, NB], F32, st2)
            Rr = P.reg("rows")
            rr = dict(reads=[Rr], writes=[Rr])
            for t5 in range(S // 512):
                tsl = slice(t5 * 512, (t5 + 1) * 512)
                rx = [P.R_xT[4 * t5 + j] for j in range(4)]
                p, Rp = fm()
                mm_group(P, p[0:4, :], [(Wg[:, dc, 0:4], P.xT[:, dc, tsl]) for dc in range(8)], rx + [R_w], [Rp])
                fw.op(fw.act, A.activation, reads=[Rp, R_w], writes=[Rr], out=T1[:, tsl], in_=p[0:4, :], func=AF.Identity, bias=bg[:, 0:1])
                p, Rp = fm()
                mm_group(P, p[0:4, :], [(Wg[:, dc, 4:8], P.xT[:, dc, tsl]) for dc in range(8)], rx + [R_w], [Rp])
                fw.op(fw.act, A.activation, reads=[Rp, R_w], writes=[Rr], out=T2[:, tsl], in_=p[0:4, :], func=AF.Exp, scale=-1.0, bias=nbf[:])
            fw.op(fw.act, A.activation, **rr, out=T2[:], in_=T2[:], func=AF.Ln, bias=1.0)
            fw.op(fw.act, A.mul, **rr, out=T2[:], in_=T2[:], mul=-1.0)
            for b in range(NB):
                bs = slice(b * 128, (b + 1) * 128)
                fw.op(fw.dve, V.tensor_tensor_scan, reads=[Rr, R_w], writes=[Rr], out=T3[:, bs], data0=ones4[:], data1=T2[:, bs], initial=0.0,
                      op0=ALU.mult, op1=ALU.add)
            fw.op(fw.dve, V.tensor_tensor_scan, **rr, out=T4[:], data0=T2[:], data1=T1[:], initial=-1e30, op0=ALU.add, op1=ALU.max)
            last = lambda T: T[:].rearrange("h (b t) -> h b t", t=128)[:, :, 127]
            fw.op(fw.dve, V.tensor_copy, **rr, out=mnew[:], in_=last(T4))
            fw.op(fw.dve, V.tensor_copy, **rr, out=blast[:], in_=last(T3))
            fw.op(fw.dve, V.memset, **rr, ap=mprev[:, 0:1], constant=-1e30)
            if NB > 1:
                fw.op(fw.dve, V.tensor_copy, **rr, out=mprev[:, 1:NB], in_=mnew[:, 0:NB - 1])
            fw.op(fw.dve, V.tensor_tensor, **rr, out=T2[:], in0=T3[:], in1=T4[:], op=ALU.subtract)
            fw.dma(fw.sp, fw.dma_clock("a_out"), a_dram, T2[:], reads=[Rr], writes=[R_cols])
            fw.op(fw.dve, V.tensor_tensor, **rr, out=T1[:], in0=T1[:], in1=T3[:], op=ALU.subtract)
            v3 = lambda T: T[:].rearrange("h (b t) -> h b t", t=128)
            bc = lambda t_: t_[:].unsqueeze(2).to_broadcast([4, NB, 128])
            fw.op(fw.dve, V.tensor_tensor, **rr, out=v3(T2), in0=v3(T2), in1=bc(mprev), op=ALU.add)
            fw.op(fw.dve, V.tensor_scalar_max, **rr, out=T2[:], in0=T2[:], scalar1=NEG)
            fw.op(fw.act, A.activation, **rr, out=T2[:], in_=T2[:], func=AF.Exp)
            fw.op(fw.act, A.activation, **rr, out=T4[:], in_=T4[:], func=AF.Exp, scale=-1.0)
            fw.op(fw.dve, V.tensor_tensor, **rr, out=dlt[:], in0=blast[:], in1=mnew[:], op=ALU.subtract)
            fw.op(fw.dve, V.tensor_tensor, **rr, out=wold[:], in0=dlt[:], in1=mprev[:], op=ALU.add)
            fw.op(fw.dve, V.tensor_scalar_max, **rr, out=wold[:], in0=wold[:], scalar1=NEG)
            fw.op(fw.act, A.activation, **rr, out=wold[:], in_=wold[:], func=AF.Exp)
            fw.op(fw.dve, V.tensor_scalar_add, **rr, out=dlt[:], in0=dlt[:], scalar1=-float(np.log(16.0)))
            fw.op(fw.dve, V.tensor_tensor, **rr, out=v3(T3), in0=v3(T1), in1=bc(dlt), op=ALU.add)
            fw.op(fw.act, A.activation, **rr, out=T3[:], in_=T3[:], func=AF.Exp)
            pcol = pSQ[:].rearrange("p (b q) -> p b q", q=16)
            for b in range(NB):
                bs = slice(b * 128, (b + 1) * 128)
                for qi, T in enumerate([T1, T2, T4, T3]):
                    fw.op(fw.pe, nc.tensor.transpose, reads=[Rr, P.R_const], writes=[R_pSQ], out=pcol[:, b, 4 * qi:4 * qi + 4], in_=T[0:4, bs],
                          identity=P.identf[0:4, 0:4])
            fw.op(fw.dve, V.tensor_copy, reads=[R_pSQ], writes=[R_cols], out=cols[:], in_=pcol[:, 0:NB, :])
            for h in range(H):
                mm_group(P, pSC[:, 0:NB], [(sel[0:4, h, :], wold[0:4, :])], [Rr, R_w], [R_pSC])
                fw.op(fw.dve, V.tensor_copy, reads=[R_pSC], writes=[R_wold], out=woldbc[:, h, :], in_=pSC[:, 0:NB])
            P.phase_barrier()
        Wout = sb("mWout", [128, 8, D], BF16)
        lng = sb("m_lng", [128, D], F32)
        lnb = sb("m_lnb", [128, D], F32)
        fw.dma(fw.sp, wclk, lng[:], P.dram["ln_g"][ln_idx // 2, ln_idx % 2, :].partition_broadcast(128), writes=[R_lnp])
        fw.dma(fw.sp, wclk, lnb[:], P.dram["ln_b"][ln_idx // 2, ln_idx % 2, :].partition_broadcast(128), writes=[R_lnp])
        z = sb("m_z", [128, D], F32); R_z = P.reg()
        tiles = {"z": [z, z], "R_z": [R_z, R_z], "zclk": [fw.dma_clock("mz")] * 2}
        for vc in range(8):
            fw.dma(fw.sp, tiles["zclk"][0], z[:], P.dram["mlstm_w_out"][0, vc * 128:(vc + 1) * 128, :], writes=[R_z])
            fw.op(fw.act, A.activation, reads=[R_z, R_w], writes=[R_w], out=Wout[:, vc, :], in_=z[:], func=AF.Identity, scale=ngc[:, vc:vc + 1])
        C32 = sb("mC32", [128, H, 2, DH], F32); C16 = sb("mC16", [128, H, 2, DH], BF16)
        n32 = sb("mn32", [128, H, 2], F32); n16 = sb("mn16", [128, H, 2], BF16)
        R_C32, R_C16 = P.regs(H), P.regs(H)
        halo = sb("m_halo", [128, 16, 3], F32); R_halo = P.regs(16)
        for h in range(H):
            fw.op(fw.pool, G_.memset, writes=[R_C32[h]], ap=C32[:, h], constant=0.0)
            fw.op(fw.pool, G_.memset, writes=[R_C16[h]], ap=C16[:, h], constant=0.0)
            fw.op(fw.pool, G_.memset, writes=[R_C32[h]], ap=n32[:, h, :], constant=0.0)
            fw.op(fw.pool, G_.memset, writes=[R_C16[h]], ap=n16[:, h, :], constant=0.0)
        for ch in range(16):
            fw.op(fw.pool, G_.memset, writes=[R_halo[ch]], ap=halo[:, ch, :], constant=0.0)
        qk = sb("m_qk", [128, 16, GRP], BF16); R_qk = P.regs(16)
        pre2 = [sb(f"m_pre{i}", [128, GRP + 4], F32) for i in range(2)]; R_pre2 = P.regs(2)
        acc2 = [sb(f"m_acc{i}", [128, GRP], F32) for i in range(2)]; R_acc2 = P.regs(2)
        v_sb = sb("m_v", [128, D], BF16); R_v = P.reg()
        og = [sb(f"m_og{i}", [128, D], BF16) for i in range(2)]; R_og = P.regs(2)
        tho = sb("m_tho", [128, D], BF16); R_tho = P.reg()
        ogT = sb("m_ogT", [128, 8, 128], BF16); R_ogT = P.reg()
        abc2 = [sb(f"m_abc{i}", [128, H, 128], F32) for i in range(2)]; R_abc2 = P.regs(2); abclk2 = [fw.dma_clock(f"abc{i}") for i in range(2)]

        def load_abc(blk):
            if blk < NB:
                k = blk % 2
                fw.dma(fw.sp, abclk2[k], abc2[k][:], a_dram[:, blk * 128:(blk + 1) * 128].partition_broadcast(128), reads=[R_cols], writes=[R_abc2[k]])
        _pT = sb("m_pT", [128, 128], F32); pT = [_pT, _pT]; _r = P.reg(); R_pT = [_r, _r]
        _sT = sb("m_sT", [128, 128], BF16); sT = [_sT, _sT]; _r = P.reg(); R_sT = [_r, _r]
        _qc = sb("m_qc", [128, DH], F32); qc = [_qc, _qc]; _r = P.reg(); R_qc = [_r, _r]
        sm = sb("m_sm", [128, H, 2], F32); den = sb("m_den", [128, H], F32); R_sm = P.regs(H); R_den = P.reg()
        R_sc = [R_pSC, R_pSC]; R_smc = [R_pSC] * H; R_SV = R_pSQ; R_QC = R_pSQ
        num = sb("m_num", [128, H, DH], F32); R_num = P.regs(H)
        _kw = sb("m_kw", [128, 2, 128], BF16); kw = [_kw, _kw]; _r = P.reg(); R_kw = [_r, _r]
        hst = sb("m_hst", [128, H, 6], F32); hmv = sb("m_hmv", [128, H, 2], F32); hsc = sb("m_hsc", [128, H], F32); R_hn = P.reg()

        nbg = GRP // 128

        def group_stage(g):
            tok0 = g * GRP
            rx = [P.R_xT[nbg * g + j] for j in range(nbg)]
            def conv_a1(ch):
                k = ch % 2
                pre, R_pre, acc, R_acc = pre2[k], R_pre2[k], acc2[k], R_acc2[k]
                p, Rp = fm()
                mm_group(P, p[:, 0:GRP], [(Win[:, dc, ch * 128:(ch + 1) * 128], P.xT[:, dc, tok0:tok0 + GRP]) for dc in range(8)], rx + [R_w, R_win], [Rp])
                fw.op(fw.act, A.copy, reads=[Rp], writes=[R_pre], out=pre[:, 3:3 + GRP], in_=p[:, 0:GRP])
                fw.op(fw.act, A.activation, reads=[Rp, R_w], writes=[R_acc], out=acc[:], in_=p[:, 0:GRP], func=AF.Identity, scale=cwh[:, ch, 3:4], bias=cbh[:, ch:ch + 1])
                fw.op(fw.pool, G_.tensor_copy, reads=[R_halo[ch]], writes=[R_pre], out=pre[:, 0:3], in_=halo[:, ch, :])

            def conv_a2(ch):
                k = ch % 2
                pre, R_pre, acc, R_acc = pre2[k], R_pre2[k], acc2[k], R_acc2[k]
                for tap in (2, 1, 0):
                    fw.op(fw.dve, V.scalar_tensor_tensor, reads=[R_pre, R_w, R_acc], writes=[R_acc], out=acc[:], in0=pre[:, tap:tap + GRP],
                          scalar=cwh[:, ch, tap:tap + 1], in1=acc[:], op0=ALU.mult, op1=ALU.add)
                fw.op(fw.pool, G_.tensor_copy, reads=[R_pre], writes=[R_halo[ch]], out=halo[:, ch, :], in_=pre[:, GRP:GRP + 3])
                fw.op(fw.act, A.activation, reads=[R_acc], writes=[R_qk[ch]], out=qk[:, ch, :], in_=acc[:], func=AF.Silu)

            def conv_b(ch):
                pass

            for ch in range(18):
                if ch >= 2:
                    conv_b(ch - 2)
                if ch < 16:
                    conv_a1(ch)
                if 1 <= ch < 17:
                    conv_a2(ch - 1)

        def front(blk):
            if True:
                g, j = divmod(blk, nbg)
                if j == 0:
                    group_stage(g)
                tok0 = g * GRP
                ogb, R_ogb = og[blk % 2], R_og[blk % 2]
                ts = slice(tok0 + j * 128, tok0 + (j + 1) * 128)
                ls = slice(j * 128, (j + 1) * 128)
                rxb = [P.R_xT[blk], R_w, R_win]
                abc, R_abc = abc2[blk % 2], R_abc2[blk % 2]
                load_abc(blk + 1)
                for half in range(2):
                    mm_group(P, pTM[:, half * 512:(half + 1) * 512],
                             [(P.xT[:, dc, ts], Win[:, dc, 2048 + half * 512:2048 + (half + 1) * 512]) for dc in range(8)], rxb, [R_pTM])
                fw.op(fw.act, A.copy, reads=[R_pTM], writes=[R_v], out=v_sb[:], in_=pTM[:])
                fw.op(fw.pool, G_.tensor_tensor, reads=[R_abc, R_w], writes=[R_abc], out=abc[:], in0=abc[:], in1=tri[:].unsqueeze(1).to_broadcast([128, H, 128]),
                      op=ALU.add)

        def front_B(blk, part=None):
            if True:
                g, j = divmod(blk, nbg)
                tok0 = g * GRP
                ogb, R_ogb = og[blk % 2], R_og[blk % 2]
                abc, R_abc = abc2[blk % 2], R_abc2[blk % 2]
                ts = slice(tok0 + j * 128, tok0 + (j + 1) * 128)
                ls = slice(j * 128, (j + 1) * 128)
                rxb = [P.R_xT[blk], R_w, R_win]

                def hv(h):
                    return (slice(h * DH, (h + 1) * DH), [qk[:, 2 * h + c, ls] for c in range(2)], [qk[:, 8 + 2 * h + c, ls] for c in range(2)],
                            [R_qk[2 * h], R_qk[2 * h + 1]], [R_qk[8 + 2 * h], R_qk[8 + 2 * h + 1]])

                def S1(h):
                    hs_, qch, kch, Rq, Rk = hv(h)
                    k = h % 2
                    sc = pSC[:, 128 * k:128 * (k + 1)]
                    fw.op(fw.act, A.activation, reads=[R_abc, R_cols], writes=[R_pT[k]], out=pT[k][:], in_=abc[:, h, :], func=AF.Exp, bias=cols[:, blk, h:h + 1])
                    mm_group(P, sc, [(kch[c], qch[c]) for c in range(2)], Rq + Rk, [R_sc[k]])
                    fw.op(fw.dve, V.scalar_tensor_tensor, reads=[R_sc[k], R_pT[k]], writes=[R_sT[k]], out=sT[k][:], in0=sc, scalar=1.0 / 16.0, in1=pT[k][:],
                          op0=ALU.mult, op1=ALU.mult)
                    for c in range(2):
                        fw.op(fw.pe, nc.tensor.transpose, reads=Rk + [P.R_const], writes=[P.R_pTR], out=P.pTR[:, c, :], in_=kch[c], identity=P.identb[:])
                    fw.op(fw.act, A.activation, reads=[P.R_pTR, R_cols], writes=[R_kw[k]], out=kw[k][:], in_=P.pTR[:, 0:2, :], func=AF.Identity,
                          scale=cols[:, blk, 12 + h:13 + h])

                def S2(h):
                    hs_, qch, kch, Rq, Rk = hv(h)
                    k = h % 2
                    c0 = 256 + 4 * h
                    mm_group(P, pSC[:, c0:c0 + 1], [(sT[k][:], onesb[:, 0:1])], [R_sT[k], R_w], [R_smc[h]])
                    mm_group(P, pSC[:, c0 + 1:c0 + 2], [(qch[c], n16[:, h, c:c + 1]) for c in range(2)], Rq + [R_C16[h]], [R_smc[h]])
                    mm_group(P, pSQ[:, 0:256], [(sT[k][:], v_sb[:, hs_])], [R_sT[k], R_v], [R_SV])
                    mm_group(P, pSQ[:, 256:512], [(qch[c], C16[:, h, c, :]) for c in range(2)], Rq + [R_C16[h]], [R_QC])
                    fw.op(fw.act, A.activation, reads=[R_QC, R_cols], writes=[R_qc[k]], out=qc[k][:], in_=pSQ[:, 256:512], func=AF.Identity,
                          scale=cols[:, blk, 4 + h:5 + h])
                    fw.op(fw.act, A.copy, reads=[R_smc[h]], writes=[R_sm[h]], out=sm[:, h, :], in_=pSC[:, c0:c0 + 2])
                    fw.op(fw.dve, V.tensor_tensor, reads=[R_SV, R_qc[k]], writes=[R_num[h]], out=num[:, h, :], in0=pSQ[:, 0:256], in1=qc[k][:], op=ALU.add)
                    fw.op(fw.dve, V.bn_stats, reads=[R_num[h]], writes=[R_hn], out=hst[:, h, :], in_=num[:, h, :])
                    for c in range(2):
                        mm_group(P, pCU[:, c, :], [(kw[k][:, c, :], v_sb[:, hs_])], [R_kw[k], R_v], [R_pCU])
                        mm_group(P, pSC[:, c0 + 2 + c:c0 + 3 + c], [(kw[k][:, c, :], onesb[:, 0:1])], [R_kw[k], R_w], [R_smc[h]])
                    wo = woldbc[:, h, blk:blk + 1]
                    fw.op(fw.dve, V.scalar_tensor_tensor, reads=[R_C32[h], R_wold, R_pCU], writes=[R_C32[h]], out=C32[:, h].rearrange("p c d -> p (c d)"),
                          in0=C32[:, h].rearrange("p c d -> p (c d)"), scalar=wo, in1=pCU[:].rearrange("p c d -> p (c d)"), op0=ALU.mult, op1=ALU.add)
                    fw.op(fw.dve, V.scalar_tensor_tensor, reads=[R_C32[h], R_wold, R_smc[h]], writes=[R_C32[h]], out=n32[:, h, :], in0=n32[:, h, :], scalar=wo,
                          in1=pSC[:, c0 + 2:c0 + 4], op0=ALU.mult, op1=ALU.add)
                    fw.op(fw.pool, G_.tensor_copy, reads=[R_C32[h]], writes=[R_C16[h]], out=C16[:, h].rearrange("p c d -> p (c d)"), in_=C32[:, h].rearrange("p c d -> p (c d)"))
                    fw.op(fw.pool, G_.tensor_copy, reads=[R_C32[h]], writes=[R_C16[h]], out=n16[:, h, :], in_=n32[:, h, :])

                if HEAD_SKEW:
                    for r in range(H + 1):
                        if r < H:
                            S1(r)
                        if r >= 1:
                            S2(r - 1)
                elif part == 0:
                    S1(0)
                    return
                else:
                    for r in range(H):
                        if not (part == 1 and r == 0):
                            S1(r)
                        S2(r)
                for half in range(2):
                    mm_group(P, pTM[:, half * 512:(half + 1) * 512],
                             [(P.xT[:, dc, ts], Win[:, dc, 3072 + half * 512:3072 + (half + 1) * 512]) for dc in range(8)], rxb, [R_pTM])
                fw.op(fw.act, A.activation, reads=[R_pTM], writes=[R_tho], out=tho[:], in_=pTM[:], func=AF.Tanh, scale=0.5)
                fw.op(fw.dve, V.tensor_tensor, reads=R_sm + [R_cols], writes=[R_den], out=den[:], in0=sm[:, :, 1], in1=cols[:, blk, 4:8], op=ALU.mult)
                fw.op(fw.dve, V.tensor_tensor, reads=R_sm + [R_den], writes=[R_den], out=den[:], in0=den[:], in1=sm[:, :, 0], op=ALU.add)
                fw.op(fw.dve, V.scalar_tensor_tensor, reads=[R_den], writes=[R_den], out=den[:], in0=den[:], scalar=-1.0, in1=den[:], op0=ALU.mult, op1=ALU.max)
                fw.op(fw.dve, V.tensor_tensor, reads=[R_den, R_cols], writes=[R_den], out=den[:], in0=den[:], in1=cols[:, blk, 8:12], op=ALU.max)
                fw.op(fw.dve, V.reciprocal, reads=[R_den], writes=[R_den], out=den[:], in_=den[:])
                for h in range(H):
                    fw.op(fw.dve, V.bn_aggr, reads=[R_hn], writes=[R_hn], out=hmv[:, h, :], in_=hst[:, h, :])
                fw.op(fw.dve, V.tensor_tensor, reads=[R_hn, R_den], writes=[R_hn], out=hsc[:], in0=hmv[:, :, 1], in1=den[:], op=ALU.mult)
                fw.op(fw.dve, V.tensor_tensor, reads=[R_hn, R_den], writes=[R_hn], out=hsc[:], in0=hsc[:], in1=den[:], op=ALU.mult)
                fw.op(fw.dve, V.tensor_scalar_add, reads=[R_hn], writes=[R_hn], out=hsc[:], in0=hsc[:], scalar1=LN_EPS)
                fw.op(fw.pool, G_.tensor_tensor, reads=[R_hn, P.R_const], writes=[R_hn], out=hsc[:], in0=hsc[:], in1=P.mhalf[:].to_broadcast([128, H]), op=ALU.pow)
                fw.op(fw.dve, V.tensor_tensor, reads=[R_hn, R_den], writes=[R_hn], out=hsc[:], in0=hsc[:], in1=den[:], op=ALU.mult)
                for h in range(H):
                    fw.op(fw.dve, V.tensor_scalar, reads=[R_num[h], R_hn], writes=[R_num[h]], out=num[:, h, :], in0=num[:, h, :], scalar1=hmv[:, h, 0:1],
                          scalar2=hsc[:, h:h + 1], op0=ALU.subtract, op1=ALU.mult)
                fw.op(fw.dve, V.scalar_tensor_tensor, reads=[R_tho] + R_num, writes=[R_ogb], out=ogb[:], in0=tho[:], scalar=1.0,
                      in1=num[:].rearrange("p h d -> p (h d)"), op0=ALU.add, op1=ALU.mult)

        def tailA(blk):
            if True:
                ogb, R_ogb = og[blk % 2], R_og[blk % 2]
                for c in range(8):
                    fw.op(fw.pe, nc.tensor.transpose, reads=[R_ogb, P.R_const], writes=[P.R_pTR], out=P.pTR[:, c, :], in_=ogb[:, c * 128:(c + 1) * 128],
                          identity=P.identb[:])
                fw.op(fw.act, A.copy, reads=[P.R_pTR], writes=[R_ogT], out=ogT[:], in_=P.pTR[:])
                for half in range(2):
                    mm_group(P, pTM[:, half * 512:(half + 1) * 512],
                             [(ogT[:, vc, :], Wout[:, vc, half * 512:(half + 1) * 512]) for vc in range(8)], [R_ogT, R_w], [R_pTM])
                residual_ln_out(P, tiles, pTM[:], R_pTM, x_src, dst_h, blk, ln_idx, lng, lnb, R_lnp, defer_cast=True)

        load_abc(0)
        for b in range(NB + 1):
            if b < NB:
                front(b)
            if 0 <= b - 1 < NB:
                tailA(b - 1)
            if b < NB:
                front_B(b)
            if 0 <= b - 1 < NB:
                zb_cast(P, z, R_z, b - 1)
                xT_finish(P, b - 1)
        P.phase_barrier()


S_FULL = 4096
T_MOE = 2048
_IN_SHAPES = {
    "gla_w_in": [1, D, 3088], "gla_w_gate_up": [1, 16, 512], "gla_norm_g": [1, D], "gla_w_out": [1, D, D],
    "mlstm_w_in": [1, D, 4104], "mlstm_w_out": [1, D, D],
    "router_w": [D, NE], "moe_w1": [2, NE, D, DFF], "moe_w3": [2, NE, D, DFF], "moe_w2": [2, NE, DFF, D],
    "ln_g": [2, 2, D], "ln_b": [2, 2, D],
    "l_gla_b_gate": [128, 4], "l_mb": [4, 2], "l_conv_w": [128, 16, 4], "l_conv_b": [128, 16], "l_mnormg": [128, 8],
    "c_tri": [128, 128], "c_trineg": [128, 128], "c_rmask": [128, 512], "c_sel": [4, 4, 128],
}
_NTL = S_FULL // TSL + 3
_SP_SHAPES = {
    "c_upper": ([128, 128], BF16), "c_ones16": ([128, 128], BF16), "c_thrp": ([128, 4, S_FULL // TSL], F32), "c_thrt": ([128, _NTL, 3], F32),
    "c_tokid": ([128, S_FULL // 128], I32), "c_oob": ([128, _NTL * TSL // 128], I32), "c_wb": ([128, 4, 8], F32), "c_w2b": ([128, 4, 4], F32),
}


def build_program(S=S_FULL, T=T_MOE):
    P = Prog(S)
    nc, fw = P.nc, P.fw
    x = P.din("x", [S, D])
    out = P.dout("out", [S, D])
    for k, (shp, dt_) in _SP_SHAPES.items():
        P.din(k, shp, dt_)
    for k, shp in _IN_SHAPES.items():
        if k not in ("ln_g", "ln_b"):
            P.din(k, shp)
    P.din("ln_g", [2, 2, D])
    P.din("ln_b", [2, 2, D])
    hA = P.dscr("hA", [S + 1, D])
    setup_common(P)
    P.bchk = nc.gpsimd.alloc_register("bchk")
    nc.gpsimd.reg_mov(P.bchk, S - 1)
    with ExitStack() as st0:
        zrow = P.sb("zrow", [1, D], F32, st0)
        Rz0 = P.reg()
        fw.op(fw.pool, nc.gpsimd.memset, writes=[Rz0], ap=zrow[:], constant=0.0)
        fw.dma(fw.sp, fw.dma_clock("zrow"), hA[S:S + 1, :], zrow[:], reads=[Rz0])
        P.phase_barrier()
    gla_phase(P, x, hA, 0, p0_src=x)
    moe_sparse_phase(P, 0, hA, hA, True)
    mlstm_phase(P, hA, hA, 2)
    moe_sparse_phase(P, 1, hA, out, False)
    fw.wait_all(fw.sp, list(fw._dmaclk.values()))
    fw.wait_all(fw.pool, list(fw._dmaclk.values()))
    return P


def host_consts(inputs):
    f32 = np.float32
    sel = np.zeros((4, 4, 128), f32)
    for h in range(4):
        sel[h, h, :] = 1.0
    rmask = np.ones((128, 512), f32)
    rmask[:, ::128] = 0.0
    c = {
        "c_identb": np.eye(128).astype(ml_dtypes.bfloat16),
        "c_identf": np.eye(128, dtype=f32),
        "c_tri": (np.arange(128)[:, None] <= np.arange(128)[None, :]).astype(f32),
        "c_rmask": rmask,
        "c_trineg": np.where(np.arange(128)[:, None] <= np.arange(128)[None, :], 0.0, -1e30).astype(f32),
        "c_sel": sel,
        "c_upper": (np.arange(128)[:, None] < np.arange(128)[None, :]).astype(ml_dtypes.bfloat16),
        "c_ones16": np.ones((128, 128), ml_dtypes.bfloat16),
        "c_thrp": np.broadcast_to((float(TSL) * np.arange(S_FULL // TSL))[None, None, :], (128, 4, S_FULL // TSL)).astype(f32).copy(),
        "c_thrt": np.broadcast_to((float(TSL) * np.arange(_NTL))[None, :, None], (128, _NTL, 3)).astype(f32).copy(),
        "c_tokid": (np.arange(S_FULL // 128)[None, :] * 128 + np.arange(128)[:, None]).astype(np.int32),
        "c_oob": np.full((128, _NTL * TSL // 128), S_FULL, np.int32),
        "c_wb": (np.arange(4)[None, :, None] * 1024 + np.arange(8)[None, None, :] * 128 + np.arange(128)[:, None, None]).astype(f32),
        "c_w2b": (np.arange(4)[None, :, None] * 512 + np.arange(4)[None, None, :] * 128 + np.arange(128)[:, None, None]).astype(f32),
        "l_gla_b_gate": np.ascontiguousarray(np.asarray(inputs["gla_b_gate"], f32)[0].reshape(4, 128).T),
        "l_mb": np.ascontiguousarray(np.asarray(inputs["mlstm_b_gates"], f32)[0].reshape(2, 4).T),
        "l_conv_w": np.ascontiguousarray(np.asarray(inputs["mlstm_conv_w"], f32)[0].reshape(4, 16, 128).transpose(2, 1, 0)),
        "l_conv_b": np.ascontiguousarray(np.asarray(inputs["mlstm_conv_b"], f32)[0].reshape(16, 128).T),
        "l_mnormg": np.ascontiguousarray(np.asarray(inputs["mlstm_norm_g"], f32)[0].reshape(8, 128).T),
    }
    return c


_PROG_CACHE = {}


def kernel(**inputs):
    x = np.asarray(inputs["x"], np.float32)
    B = x.shape[0]
    if "p" not in _PROG_CACHE:
        _PROG_CACHE["p"] = build_program()
    P = _PROG_CACHE["p"]
    shared = {k: np.ascontiguousarray(np.asarray(inputs[k], np.float32)) for k in
              ["gla_w_in", "gla_w_gate_up", "gla_norm_g", "gla_w_out", "mlstm_w_in", "mlstm_w_out", "router_w", "moe_w1", "moe_w3", "moe_w2", "ln_g", "ln_b"]}
    shared.update(host_consts(inputs))
    in_maps = [dict(shared, x=np.ascontiguousarray(x[b])) for b in range(B)]
    res = run_bass_kernel_spmd(P.nc, in_maps, core_ids=list(range(B)))
    return np.stack([np.asarray(r["out"], np.float32)[:S_FULL] for r in res.results], axis=0)


def _idma(fw, clk, out, in_, out_offset=None, in_offset=None, reads=(), writes=(), bounds_check=None):
    if not clk.name.endswith("_sw"):
        clk = fw.dma_clock(clk.name[2:] + "_sw")
    fw._deps(fw.pool, reads, writes, skip_clock=clk)
    kw = {}
    if bounds_check is not None:
        kw = dict(bounds_check=bounds_check, oob_is_err=False)
    ins = fw.nc.gpsimd.indirect_dma_start(out=out, out_offset=out_offset, in_=in_, in_offset=in_offset, **kw)
    ins.then_inc(clk.sem, 16)
    clk.count += 16
    for r in reads:
        r.r[clk] = clk.count
    for w in writes:
        w.w = (clk, clk.count)
        w.r = {}
    fw.n_inst += 1
    return ins


def moe_sparse_phase(P, layer, src_h, dst_h, make_xT):
    nc, fw = P.nc, P.fw
    S, NB = P.S, P.NB
    NC = NB
    KP = S // TSL
    NTL = KP + 3
    NSL = NTL * TSL
    NJ = TSL // 128
    V, A, G_ = nc.vector, nc.scalar, nc.gpsimd
    OFF = IndirectOffsetOnAxis
    w1t = P.dram["moe_w1"].rearrange("l e d f -> (l e d) f")
    w3t = P.dram["moe_w3"].rearrange("l e d f -> (l e d) f")
    w2t = P.dram["moe_w2"].rearrange("l e f d -> (l e f) d")
    slot_tok = P.dscr(f"slot_tok{layer}", [NSL, 1], I32)
    g4_dram = P.dscr(f"g4_dram{layer}", [S + 1, 4])
    with ExitStack() as st:
        sb = lambda n, s, d: P.sb(n, s, d, st)
        widx = sb("s_widx", [128, NTL, 4, 8], I32)
        w2idx = sb("s_w2idx", [128, NTL, 4, 4], I32)
        R_idx = P.reg("widx")
        lng = sb("s_lng", [128, D], F32); lnb = sb("s_lnb", [128, D], F32); R_lnp = P.reg()
        ck = fw.dma_clock("s_const")
        fw.dma(fw.sp, ck, lng[:], P.dram["ln_g"][layer, 1, :].partition_broadcast(128), writes=[R_lnp])
        fw.dma(fw.sp, ck, lnb[:], P.dram["ln_b"][layer, 1, :].partition_broadcast(128), writes=[R_lnp])
        pA = [P.ps(f"s_pA{i}", [128, 512], F32, st) for i in range(2)]
        pB = [P.ps(f"s_pB{i}", [128, 512], F32, st) for i in range(2)]
        pY = [P.ps(f"s_pY{i}", [128, 512], F32, st) for i in range(2)]
        pR = P.ps("s_pR", [128, 512], F32, st)
        R_pA, R_pB, R_pY, R_pR = P.regs(2), P.regs(2), P.regs(2), P.reg()
        W1 = [sb(f"s_W1_{s}", [128, 8, DFF], BF16) for s in range(2)]
        W3 = [sb(f"s_W3_{s}", [128, 8, DFF], BF16) for s in range(2)]
        W2 = [sb(f"s_W2_{s}", [128, 4, D], BF16) for s in range(2)]
        R_W1, R_W3, R_W2 = [P.regs(8), P.regs(8)], [P.regs(8), P.regs(8)], [P.regs(4), P.regs(4)]
        wclk = [[fw.dma_clock(f"s_w{m}_{s}") for s in range(2)] for m in range(3)]

        def load_w(s, jj, slot, part=None):
            jobs = [(0, dc) for dc in range(8)] + [(1, dc) for dc in range(8)] + [(2, fc) for fc in range(4)]
            if part is not None:
                jobs = jobs[:10] if part == 0 else jobs[10:]
            for m, c in jobs:
                if m == 0:
                    _idma(fw, wclk[0][slot], W1[slot][:, c, :], w1t, in_offset=OFF(ap=widx[:, s, jj, c:c + 1], axis=0), reads=[R_idx], writes=[R_W1[slot][c]])
                elif m == 1:
                    _idma(fw, wclk[1][slot], W3[slot][:, c, :], w3t, in_offset=OFF(ap=widx[:, s, jj, c:c + 1], axis=0), reads=[R_idx], writes=[R_W3[slot][c]])
                else:
                    _idma(fw, wclk[2][slot], W2[slot][:, c, :], w2t, in_offset=OFF(ap=w2idx[:, s, jj, c:c + 1], axis=0), reads=[R_idx], writes=[R_W2[slot][c]])

        with ExitStack() as st2:
            sb2 = lambda n, s, d: P.sb(n, s, d, st2)
            wr = sb2("s_wr", [128, 8, NE], BF16); R_wr = P.reg()
            fw.dma(fw.pool, fw.dma_clock("s_wr"), wr[:], P.dram["router_w"].rearrange("(c p) e -> p c e", p=128), writes=[R_wr])
            aff = sb2("s_aff", [128, NC, NE], F32); am = sb2("s_am", [128, NC, NE], F32)
            k1 = sb2("s_k1", [128, NC, NE], F32); k2 = sb2("s_k2", [128, NC, NE], F32)
            t6 = sb2("s_t6", [128, NC, 4, 6], F32); gs = sb2("s_gs", [128, NC, 4], F32)
            gmax = sb2("s_gmax", [128, NC], F32); gmask = sb2("s_gmask", [128, NC, 4], F32)
            m1 = sb2("s_m1", [128, NC], F32); m2 = sb2("s_m2", [128, NC], F32)
            g4 = sb2("s_g4", [128, NC, 4], F32)
            gm16 = sb2("s_gm16", [128, NC * 4], BF16)
            upper = sb2("s_upper", [128, 128], BF16); ones16 = sb2("s_ones16", [128, 128], BF16)
            ones32 = sb2("s_ones32", [128, NC], F32)
            tot = sb2("s_tot", [128, 4, NC], F32); incl = sb2("s_incl", [128, 4, NC], F32)
            pw = sb2("s_pw", [128, NC, 4], F32)
            thrp = sb2("s_thrp", [128, 4, KP], F32); thrt = sb2("s_thrt", [128, NTL, 3], F32)
            cmpp = sb2("s_cmpp", [128, 4, KP], F32); cmpt = sb2("s_cmpt", [128, NTL, 3], F32)
            padg = sb2("s_padg", [128, 4], F32); cum = sb2("s_cum", [128, 4], F32); base = sb2("s_base", [128, 4], F32)
            gid = sb2("s_gid", [128, NTL], F32)
            posf = sb2("s_posf", [128, NC], F32); posi = sb2("s_posi", [128, NC], I32)
            tokid = sb2("s_tokid", [128, NC], I32); oobt = sb2("s_oob", [128, NSL // 128], I32)
            wb = sb2("s_wb", [128, 4, 8], F32); w2b = sb2("s_w2b", [128, 4, 4], F32)
            wf = sb2("s_wf", [128, NTL, 4, 8], F32); w2f = sb2("s_w2f", [128, NTL, 4, 4], F32)
            R_rt = P.reg("srt")
            for t_, name in [(upper, "c_upper"), (ones16, "c_ones16"), (thrp, "c_thrp"), (thrt, "c_thrt"), (tokid, "c_tokid"), (oobt, "c_oob"),
                             (wb, "c_wb"), (w2b, "c_w2b")]:
                fw.dma(fw.sp, ck, t_[:], P.dram[name], writes=[R_rt])
            fw.op(fw.pool, G_.memset, writes=[R_rt], ap=ones32[:], constant=1.0)
            sclk = fw.dma_clock("s_slot")
            fw.dma(fw.sp, sclk, slot_tok.rearrange("(p j) o -> p (j o)", p=128), oobt[:], reads=[R_rt])
            pRv = pR[:, 0:NC * NE].rearrange("p (c e) -> p c e", e=NE)
            for c in range(NC):
                mm_group(P, pRv[:, c, :], [(P.xT[:, dc, c * 128:(c + 1) * 128], wr[:, dc, :]) for dc in range(8)], [P.R_xT[c], R_wr], [R_pR])
            rt = dict(reads=[R_rt], writes=[R_rt])
            fw.op(fw.act, A.activation, reads=[R_pR], writes=[R_rt], out=aff[:], in_=pRv, func=AF.Sigmoid)
            a4 = aff[:].rearrange("p c (g k) -> p c g k", k=4)
            fw.op(fw.dve, V.tensor_tensor, **rt, out=t6[:, :, :, 0:2], in0=a4[:, :, :, 0:2], in1=a4[:, :, :, 2:4], op=ALU.add)
            fw.op(fw.dve, V.tensor_tensor, **rt, out=t6[:, :, :, 2:5], in0=a4[:, :, :, 0:3], in1=a4[:, :, :, 1:4], op=ALU.add)
            fw.op(fw.dve, V.tensor_tensor, **rt, out=t6[:, :, :, 5:6], in0=a4[:, :, :, 0:1], in1=a4[:, :, :, 3:4], op=ALU.add)
            fw.op(fw.dve, V.tensor_reduce, **rt, out=gs[:], in_=t6[:], axis=AX.X, op=ALU.max)
            fw.op(fw.dve, V.tensor_reduce, **rt, out=gmax[:], in_=gs[:], axis=AX.X, op=ALU.max)
            fw.op(fw.dve, V.tensor_tensor, **rt, out=gmask[:], in0=gs[:], in1=gmax[:].unsqueeze(2).to_broadcast([128, NC, 4]), op=ALU.is_equal)
            gm_b = gmask[:].unsqueeze(3).to_broadcast([128, NC, 4, 4])
            am4 = am[:].rearrange("p c (g k) -> p c g k", k=4)
            fw.op(fw.dve, V.tensor_tensor, **rt, out=am4, in0=a4, in1=gm_b, op=ALU.mult)
            fw.op(fw.dve, V.tensor_tensor, **rt, out=am4, in0=am4, in1=gm_b, op=ALU.add)
            fw.op(fw.dve, V.tensor_scalar_add, **rt, out=am[:], in0=am[:], scalar1=-1.0)
            fw.op(fw.dve, V.tensor_reduce, **rt, out=m1[:], in_=am[:], axis=AX.X, op=ALU.max)
            bc16 = lambda t_: t_[:].unsqueeze(2).to_broadcast([128, NC, NE])
            fw.op(fw.dve, V.tensor_tensor, **rt, out=k1[:], in0=am[:], in1=bc16(m1), op=ALU.is_equal)
            fw.op(fw.dve, V.scalar_tensor_tensor, **rt, out=am[:], in0=k1[:], scalar=-2.0, in1=am[:], op0=ALU.mult, op1=ALU.add)
            fw.op(fw.dve, V.tensor_reduce, **rt, out=m2[:], in_=am[:], axis=AX.X, op=ALU.max)
            fw.op(fw.dve, V.tensor_tensor, **rt, out=k2[:], in0=am[:], in1=bc16(m2), op=ALU.is_equal)
            fw.op(fw.dve, V.tensor_tensor, **rt, out=k1[:], in0=k1[:], in1=bc16(m1), op=ALU.mult)
            fw.op(fw.dve, V.tensor_tensor, **rt, out=k2[:], in0=k2[:], in1=bc16(m2), op=ALU.mult)
            fw.op(fw.dve, V.tensor_tensor, **rt, out=k1[:], in0=k1[:], in1=k2[:], op=ALU.add)
            fw.op(fw.dve, V.tensor_tensor, **rt, out=m1[:], in0=m1[:], in1=m2[:], op=ALU.add)
            fw.op(fw.dve, V.reciprocal, **rt, out=m1[:], in_=m1[:])
            fw.op(fw.dve, V.tensor_tensor, **rt, out=k1[:], in0=k1[:], in1=bc16(m1), op=ALU.mult)
            R_g4 = P.reg("g4")
            fw.op(fw.dve, V.tensor_reduce, reads=[R_rt], writes=[R_g4], out=g4[:], in_=k1[:].rearrange("p c (g j) -> p c j g", j=4), axis=AX.X, op=ALU.add)
            gclk = fw.dma_clock("s_g4")
            fw.dma(fw.sp, gclk, g4_dram[0:S, :].rearrange("(c p) j -> p c j", p=128), g4[:], reads=[R_g4])
            fw.dma(fw.sp, gclk, g4_dram[S:S + 1, :], lng[0:1, 0:4], reads=[R_lnp])
            fw.op(fw.dve, V.tensor_copy, **rt, out=gm16[:], in_=gmask[:].rearrange("p c g -> p (c g)"))
            mm_group(P, pR[:, 0:NC * 4], [(upper[:], gm16[:])], [R_rt], [R_pR])
            fw.op(fw.dve, V.tensor_copy, reads=[R_pR], writes=[R_rt], out=pw[:], in_=pR[:, 0:NC * 4].rearrange("p (c g) -> p c g", g=4))
            mm_group(P, pR[:, 0:NC * 4], [(ones16[:], gm16[:])], [R_rt], [R_pR])
            fw.op(fw.dve, V.tensor_copy, reads=[R_pR], writes=[R_rt], out=tot[:], in_=pR[:, 0:NC * 4].rearrange("p (c g) -> p g c", g=4))
            for g in range(4):
                fw.op(fw.dve, V.tensor_tensor_scan, **rt, out=incl[:, g, :], data0=ones32[:], data1=tot[:, g, :], initial=0.0, op0=ALU.mult, op1=ALU.add)
            ng = incl[:, :, NC - 1]
            fw.op(fw.dve, V.tensor_tensor, **rt, out=tot[:], in0=incl[:], in1=tot[:], op=ALU.subtract)
            fw.op(fw.dve, V.tensor_tensor, **rt, out=cmpp[:], in0=ng.unsqueeze(2).to_broadcast([128, 4, KP]), in1=thrp[:], op=ALU.is_gt)
            fw.op(fw.dve, V.tensor_reduce, **rt, out=padg[:], in_=cmpp[:], axis=AX.X, op=ALU.add)
            fw.op(fw.dve, V.tensor_scalar_mul, **rt, out=padg[:], in0=padg[:], scalar1=float(TSL))
            fw.op(fw.dve, V.tensor_tensor_scan, **rt, out=cum[:], data0=ones32[:, 0:4], data1=padg[:], initial=0.0, op0=ALU.mult, op1=ALU.add)
            fw.op(fw.dve, V.tensor_tensor, **rt, out=base[:], in0=cum[:], in1=padg[:], op=ALU.subtract)
            fw.op(fw.dve, V.tensor_tensor, **rt, out=pw[:], in0=pw[:], in1=tot[:].rearrange("p g c -> p c g"), op=ALU.add)
            fw.op(fw.dve, V.tensor_tensor, **rt, out=pw[:], in0=pw[:], in1=base[:].unsqueeze(1).to_broadcast([128, NC, 4]), op=ALU.add)
            fw.op(fw.dve, V.tensor_tensor, **rt, out=pw[:], in0=pw[:], in1=gmask[:], op=ALU.mult)
            fw.op(fw.dve, V.tensor_reduce, **rt, out=posf[:], in_=pw[:], axis=AX.X, op=ALU.add)
            fw.op(fw.dve, V.tensor_copy, **rt, out=posi[:], in_=posf[:])
            fw.op(fw.dve, V.tensor_tensor, **rt, out=cmpt[:], in0=cum[:, 0:3].unsqueeze(1).to_broadcast([128, NTL, 3]), in1=thrt[:], op=ALU.is_le)
            fw.op(fw.dve, V.tensor_reduce, **rt, out=gid[:], in_=cmpt[:], axis=AX.X, op=ALU.add)
            for s in range(NTL):
                fw.op(fw.dve, V.scalar_tensor_tensor, **rt, out=wf[:, s], in0=gid[:, s:s + 1].unsqueeze(2).to_broadcast([128, 4, 8]), scalar=4096.0, in1=wb[:],
                      op0=ALU.mult, op1=ALU.add)
                fw.op(fw.dve, V.scalar_tensor_tensor, **rt, out=w2f[:, s], in0=gid[:, s:s + 1].unsqueeze(2).to_broadcast([128, 4, 4]), scalar=2048.0, in1=w2b[:],
                      op0=ALU.mult, op1=ALU.add)
            if layer:
                fw.op(fw.dve, V.tensor_scalar_add, **rt, out=wf[:], in0=wf[:], scalar1=float(layer * NE * D))
                fw.op(fw.dve, V.tensor_scalar_add, **rt, out=w2f[:], in0=w2f[:], scalar1=float(layer * NE * DFF))
            fw.op(fw.dve, V.tensor_copy, reads=[R_rt], writes=[R_idx], out=widx[:], in_=wf[:])
            fw.op(fw.dve, V.tensor_copy, reads=[R_rt], writes=[R_idx], out=w2idx[:], in_=w2f[:])
            R_slot = P.reg("slot")
            fw.wait_all(fw.pool, [sclk])
            for c in range(NC):
                _idma(fw, sclk, slot_tok, tokid[:, c:c + 1], out_offset=OFF(ap=posi[:, c:c + 1], axis=0), reads=[R_rt], writes=[R_slot])
            P.phase_barrier()
        hrow = [sb(f"s_hrow{i}", [128, NJ, D], F32) for i in range(2)]; R_hrow = [P.regs(NJ), P.regs(NJ)]
        g4t = [sb(f"s_g4t{i}", [128, NJ, 4], F32) for i in range(2)]; R_g4t = [P.regs(NJ), P.regs(NJ)]
        idx = [sb(f"s_idx{i}", [128, NJ], I32) for i in range(2)]; R_ix = P.regs(2)
        iclk = [fw.dma_clock(f"s_ix{i}") for i in range(2)]
        hclk = [fw.dma_clock(f"s_h{i}") for i in range(2)]
        oclk = [fw.dma_clock(f"s_o{i}") for i in range(2)]
        hs = [sb(f"s_hs{i}", [128, 512], F32) for i in range(2)]; R_hs = P.regs(2)
        hp = [sb(f"s_hp{i}", [128, 4, 512], BF16) for i in range(2)]
        R_hp = [[P.reg() for f in range(4)] for i in range(2)]
        lnT = {"junk": sb("s_junk", [128, D], BF16), "R_junk": P.reg(), "s": [sb(f"s_lns{i}", [128, 8], F32) for i in range(2)], "R": P.regs(2), "ctr": [0]}
        xTt = [P.xT[:, :, i * TSL:(i + 1) * TSL] for i in range(2)]
        R_xt = [P.regs(NJ), P.regs(NJ)]

        def load_tile(s):
            b = s % 2
            fw.dma(fw.sp, iclk[b], idx[b][:], slot_tok[s * TSL:(s + 1) * TSL, :].rearrange("(p j) o -> p (j o)", j=NJ), writes=[R_ix[b]])
            for j in range(NJ):
                _idma(fw, hclk[b], hrow[b][:, j, :], src_h, in_offset=OFF(ap=idx[b][:, j:j + 1], axis=0), reads=[R_ix[b]], writes=[R_hrow[b][j]])
                _idma(fw, hclk[b], g4t[b][:, j, :], g4_dram, in_offset=OFF(ap=idx[b][:, j:j + 1], axis=0), reads=[R_ix[b]], writes=[R_g4t[b][j]])

        def prep_tile(s):
            b = s % 2
            for j in range(NJ):
                fw.op(fw.act, A.copy, reads=[R_hrow[b][j]], writes=[P.R_zb[0]], out=P.zb[0][:], in_=hrow[b][:, j, :])
                for c in range(8):
                    fw.op(fw.pe, nc.tensor.transpose, reads=[P.R_zb[0], P.R_const], writes=[P.R_pTR], out=P.pTR[:, c, :],
                          in_=P.zb[0][:, c * 128:(c + 1) * 128], identity=P.identb[:])
                fw.op(fw.dve, V.tensor_copy, reads=[P.R_pTR], writes=[R_xt[b][j]], out=xTt[b][:, :, j * 128:(j + 1) * 128], in_=P.pTR[:])
                fw.op(fw.act, A.mul, reads=[R_hrow[b][j]], writes=[R_hrow[b][j]], out=hrow[b][:, j, :], in_=hrow[b][:, j, :], mul=ALPHA)

        ycnt = [0]

        def H(b, t5, slot, hb):
            rx = [R_xt[b][t5 * 4 + j] for j in range(4)]
            xs = lambda dc: xTt[b][:, dc, t5 * 512:(t5 + 1) * 512]
            for fc in range(4):
                a_, b_ = pA[fc % 2], pB[fc % 2]
                mm_group(P, a_[:], [(W1[slot][:, dc, fc * 128:(fc + 1) * 128], xs(dc)) for dc in range(8)], rx + R_W1[slot], [R_pA[fc % 2]])
                mm_group(P, b_[:], [(W3[slot][:, dc, fc * 128:(fc + 1) * 128], xs(dc)) for dc in range(8)], rx + R_W3[slot], [R_pB[fc % 2]])
                fw.op(fw.act, A.activation, reads=[R_pA[fc % 2]], writes=[R_hs[fc % 2]], out=hs[fc % 2][:], in_=a_[:], func=AF.Silu)
                fw.op(fw.dve, V.tensor_tensor, reads=[R_hs[fc % 2], R_pB[fc % 2]], writes=[R_hp[hb][fc]], out=hp[hb][:, fc, :], in0=hs[fc % 2][:], in1=b_[:],
                      op=ALU.mult)

        def Y(b, t5, slot, hb, jj):
            for tc in range(4):
                j = t5 * 4 + tc
                for dh in range(2):
                    k = ycnt[0] % 2
                    ycnt[0] += 1
                    mm_group(P, pY[k][:], [(hp[hb][:, fc, tc * 128:(tc + 1) * 128], W2[slot][:, fc, dh * 512:(dh + 1) * 512]) for fc in range(4)],
                             R_hp[hb] + R_W2[slot], [R_pY[k]])
                    ysl = hrow[b][:, j, dh * 512:(dh + 1) * 512]
                    fw.op(fw.dve, V.scalar_tensor_tensor, reads=[R_pY[k], R_g4t[b][j], R_hrow[b][j]], writes=[R_hrow[b][j]], out=ysl, in0=pY[k][:],
                          scalar=g4t[b][:, j, jj:jj + 1], in1=ysl, op0=ALU.mult, op1=ALU.add)

        def finish_sub(s, j):
            b = s % 2
            z = hrow[b][:, j, :]
            _ln_two_regs(P, z, [R_hrow[b][j]], lng, lnb, R_lnp, on_dve=True)
            _idma(fw, oclk[b], dst_h[0:S, :], z, out_offset=OFF(ap=idx[b][:, j:j + 1], axis=0), reads=[R_hrow[b][j], R_ix[b]], bounds_check=P.bchk)

        wpend = []
        pending = []

        def drain(n):
            for _ in range(n):
                if not pending:
                    return
                s_, j_ = pending.pop(0)
                finish_sub(s_, j_)
                if j_ == NJ - 1 and s_ + 2 < NTL:
                    load_tile(s_ + 2)

        units = [(s, jj) for s in range(NTL) for jj in range(4)]
        load_tile(0)
        load_w(0, 0, 0)
        prep_tile(0)
        load_w(0, 1, 1)
        load_tile(1)
        prev = None
        hb = 0
        for u, (s, jj) in enumerate(units):
            b, slot = s % 2, u % 2
            if jj == 3 and s + 1 < NTL:
                prep_tile(s + 1)
            for t5 in range(NJ // 4):
                H(b, t5, slot, hb)
                if prev is not None:
                    Y(*prev[:5])
                    if prev[5] and units[prev[6]][1] == 3:
                        pending.extend((units[prev[6]][0], j) for j in range(NJ))
                    if prev[5]:
                        pu = prev[6]
                        if pu + 2 < len(units):
                            load_w(units[pu + 2][0], units[pu + 2][1], pu % 2)
                    drain(2)
                last = (t5 == NJ // 4 - 1)
                prev = (b, t5, slot, hb, jj, last, u)
                hb ^= 1
        Y(*prev[:5])
        pending.extend((NTL - 1, j) for j in range(NJ))
        drain(len(pending))
        P.phase_barrier()
    if make_xT:
        with ExitStack() as st:
            NXB = 6
            xin = [P.sb(f"s_xin{i}", [128, D], F32, st) for i in range(NXB)]
            R_xin = [[P.reg()] for i in range(NXB)]
            clk = [fw.dma_clock(f"s_xin{i}") for i in range(NXB)]
            for bb in range(NB):
                k = bb % NXB
                fw.dma(fw.sp, clk[k], xin[k][:], dst_h[bb * 128:(bb + 1) * 128, :], writes=R_xin[k])
                _to_xT_two(P, xin[k][:], R_xin[k], bb)
            P.phase_barrier()
```

```python
import numpy as np
import ml_dtypes
from contextlib import ExitStack
import concourse.bass as bass
import concourse.mybir as mybir
from concourse.bass import IndirectOffsetOnAxis
from concourse.bass_utils import run_bass_kernel_spmd

F32 = mybir.dt.float32
BF16 = mybir.dt.bfloat16
AF = mybir.ActivationFunctionType
ALU = mybir.AluOpType
AX = mybir.AxisListType

D = 1024
ALPHA = (2 * 2) ** 0.25
LN_EPS = 1e-5
NE = 16
DFF = 512
import os
HEAD_SKEW = os.environ.get('HEAD_SKEW', '0') == '1'
I32 = mybir.dt.int32
TSL = 1024


class Reg:
    __slots__ = ("name", "w", "r")

    def __init__(self, name=""):
        self.name = name
        self.w = None
        self.r = {}


class Clock:
    def __init__(self, fw, name, step):
        self.name = name
        self.step = step
        self.sem = fw.stack.enter_context(fw.nc.semaphore(name))
        self.count = 0
        self.is_dma = step == 16


class Eng:
    def __init__(self, fw, name, eng, pe=False):
        self.name = name
        self.eng = eng
        self.clock = Clock(fw, "s_" + name, 1)
        self.seen = {}
        self.pe = pe


class FW:
    def __init__(self, nc, stack):
        self.nc = nc
        self.stack = stack
        self.pe = Eng(self, "pe", nc.tensor, pe=True)
        self.act = Eng(self, "act", nc.scalar)
        self.dve = Eng(self, "dve", nc.vector)
        self.pool = Eng(self, "pool", nc.gpsimd)
        self.sp = Eng(self, "sp", nc.sync)
        self.n_inst = 0
        self.n_wait = 0
        self._dmaclk = {}

    def dma_clock(self, name):
        if name not in self._dmaclk:
            self._dmaclk[name] = Clock(self, "d_" + name, 16)
        return self._dmaclk[name]

    def _deps(self, E, reads, writes, skip_clock=None):
        deps = {}

        def add(cv):
            c, v = cv
            if deps.get(c, 0) < v:
                deps[c] = v
        for r in reads:
            if r.w is not None:
                add(r.w)
        for w in writes:
            if w.w is not None and w.w[0] is not skip_clock:
                add(w.w)
            for cv in w.r.items():
                add(cv)
        for c, v in deps.items():
            if c.is_dma:
                v = c.count
            if c is E.clock and E.pe:
                continue
            if E.seen.get(c, 0) >= v:
                continue
            E.eng.wait_ge(c.sem, v)
            E.seen[c] = v
            self.n_wait += 1

    def op(self, E, fn, reads=(), writes=(), sig=True, **kw):
        self._deps(E, reads, writes)
        ins = fn(**kw)
        if sig:
            ins.then_inc(E.clock.sem, 1)
            E.clock.count += 1
            cnt = E.clock.count
        else:
            cnt = E.clock.count + 1
        for r in reads:
            r.r[E.clock] = cnt
        for w in writes:
            w.w = (E.clock, cnt)
            w.r = {}
        self.n_inst += 1
        return ins

    def dma(self, Q, clk, out, in_, reads=(), writes=(), **kw):
        if Q is self.pool and not clk.name.endswith("_sw"):
            clk = self.dma_clock(clk.name[2:] + "_sw")
        self._deps(Q, reads, writes, skip_clock=clk)
        ins = Q.eng.dma_start(out=out, in_=in_, **kw)
        ins.then_inc(clk.sem, 16)
        clk.count += 16
        for r in reads:
            r.r[clk] = clk.count
        for w in writes:
            w.w = (clk, clk.count)
            w.r = {}
        self.n_inst += 1
        return ins

    def wait_all(self, E, clocks):
        for c in clocks:
            if c.count > 0 and E.seen.get(c, 0) < c.count:
                E.eng.wait_ge(c.sem, c.count)
                E.seen[c] = c.count


class Prog:
    def __init__(self, S):
        self.S = S
        self.NB = S // 128
        self.nc = bass.Bass("TRN2", target_bir_lowering=False)
        self.stack = ExitStack()
        self.fw = FW(self.nc, self.stack)
        self.dram = {}
        self._uid = 0

    def din(self, name, shape, dt=F32):
        t = self.nc.dram_tensor(name, list(shape), dt, kind="ExternalInput").ap()
        self.dram[name] = t
        return t

    def dout(self, name, shape, dt=F32):
        t = self.nc.dram_tensor(name, list(shape), dt, kind="ExternalOutput").ap()
        self.dram[name] = t
        return t

    def dscr(self, name, shape, dt=F32):
        t = self.nc.dram_tensor(name, list(shape), dt, kind="Internal").ap()
        self.dram[name] = t
        return t

    def sb(self, name, shape, dt, stack=None):
        self._uid += 1
        return (stack or self.stack).enter_context(self.nc.sbuf_tensor(f"{name}_{self._uid}", list(shape), dt))

    def ps(self, name, shape, dt, stack=None):
        self._uid += 1
        return (stack or self.stack).enter_context(self.nc.psum_tensor(f"{name}_{self._uid}", list(shape), dt))

    def reg(self, name=""):
        return Reg(name)

    def regs(self, n, name=""):
        return [Reg(f"{name}{i}") for i in range(n)]


def setup_common(P):
    nc, fw = P.nc, P.fw
    S, NB = P.S, P.NB
    P.xT = P.sb("xT", [128, 8, S], BF16)
    P.R_xT = P.regs(NB, "xT")
    P.identb = P.sb("identb", [128, 128], BF16)
    P.identf = P.sb("identf", [128, 128], F32)
    P.R_const = P.reg("const")
    c_identb = P.din("c_identb", [128, 128], BF16)
    c_identf = P.din("c_identf", [128, 128], F32)
    clk = fw.dma_clock("const")
    fw.dma(fw.sp, clk, P.identb[:], c_identb, writes=[P.R_const])
    fw.dma(fw.sp, clk, P.identf[:], c_identf, writes=[P.R_const])
    P.mhalf = P.sb("mhalf", [128, 1], F32)
    fw.op(fw.pool, nc.gpsimd.memset, writes=[P.R_const], ap=P.mhalf[:], constant=-0.5)
    P.ln_stats = [P.sb(f"ln_stats{i}", [128, 2, 6], F32) for i in range(2)]
    P.ln_mv = [P.sb(f"ln_mv{i}", [128, 2], F32) for i in range(2)]
    P.ln_rstd = [P.sb(f"ln_rstd{i}", [128, 1], F32) for i in range(2)]
    P.R_ln = [P.reg(f"ln{i}") for i in range(2)]
    _zb = P.sb("zb", [128, D], BF16)
    P.zb = [_zb, _zb]
    _rzb = P.reg("zb")
    P.R_zb = [_rzb, _rzb]
    P.ln_ctr = 0
    P.pTR = P.ps("pTR", [128, 8, 128], BF16)
    P.R_pTR = P.reg("pTR")


def moe_phase(P, layer, src_h, dst_h, make_xT, T):
    nc, fw = P.nc, P.fw
    S = P.S
    NC_T = T // 128
    NT5 = T // 512
    ln_idx = 2 * layer + 1
    w1 = P.dram["moe_w1"]
    w3 = P.dram["moe_w3"]
    w2 = P.dram["moe_w2"]
    with ExitStack() as st:
        yacc = P.sb("yacc", [128, NC_T, D], F32, st)
        R_y = [[P.reg(f"y{c}_{h}") for h in range(2)] for c in range(NC_T)]
        W1 = [P.sb(f"W1_{s}", [128, 8, DFF], BF16, st) for s in range(2)]
        W3 = [P.sb(f"W3_{s}", [128, 8, DFF], BF16, st) for s in range(2)]
        W2 = [P.sb(f"W2_{s}", [128, 4, D], BF16, st) for s in range(2)]
        R_W1, R_W3, R_W2 = P.regs(2, "W1"), P.regs(2, "W3"), P.regs(2, "W2")
        wclk = [[fw.dma_clock(f"w{m}_{s}") for s in range(2)] for m in range(3)]
        wr = P.sb("wr", [128, 8, NE], BF16, st)
        R_wr = P.reg("wr")
        aff = P.sb("aff", [128, NC_T, NE], F32, st)
        t6 = P.sb("t6", [128, NC_T, 4, 6], F32, st)
        gs = P.sb("gs", [128, NC_T, 4], F32, st)
        gmax = P.sb("gmax", [128, NC_T], F32, st)
        gmask = P.sb("gmask", [128, NC_T, 4], F32, st)
        am = P.sb("am", [128, NC_T, NE], F32, st)
        m1 = P.sb("m1", [128, NC_T], F32, st)
        m2 = P.sb("m2", [128, NC_T], F32, st)
        k1 = P.sb("k1", [128, NC_T, NE], F32, st)
        gates = k1
        k2 = P.sb("k2", [128, NC_T, NE], F32, st)
        R_rt = P.reg("router")
        hs = [P.sb(f"hs{i}", [128, 512], F32, st) for i in range(2)]
        R_hs = P.regs(2, "hs")
        hp = [P.sb(f"hp{i}", [128, 4, 512], BF16, st) for i in range(2)]
        R_hp = [[P.reg(f"hp{i}_{f}") for f in range(4)] for i in range(2)]
        pA = [P.ps(f"pA{i}", [128, 512], F32, st) for i in range(2)]
        pB = [P.ps(f"pB{i}", [128, 512], F32, st) for i in range(2)]
        pY = [P.ps(f"pY{i}", [128, 512], F32, st) for i in range(2)]
        pR = P.ps("pR", [128, NC_T, NE], F32, st)
        R_pA, R_pB, R_pY, R_pR = P.regs(2, "pA"), P.regs(2, "pB"), P.regs(2, "pY"), P.reg("pR")
        yclk = fw.dma_clock("yio")
        lng = P.sb("e_lng", [128, D], F32, st)
        lnb = P.sb("e_lnb", [128, D], F32, st)
        R_lnp = P.reg("elnp")
        fw.dma(fw.sp, fw.dma_clock("elnp"), lng[:], P.dram["ln_g"][layer, 1, :].partition_broadcast(128), writes=[R_lnp])
        fw.dma(fw.sp, fw.dma_clock("elnp"), lnb[:], P.dram["ln_b"][layer, 1, :].partition_broadcast(128), writes=[R_lnp])

        fw.dma(fw.pool, fw.dma_clock("wr"), wr[:], P.dram["router_w"].rearrange("(c p) e -> p c e", p=128), writes=[R_wr])

        def load_w(e, s):
            fw.dma(fw.pool, wclk[0][s], W1[s][:], w1[layer, e].rearrange("(c p) f -> p c f", p=128), writes=[R_W1[s]])
            fw.dma(fw.pool, wclk[1][s], W3[s][:], w3[layer, e].rearrange("(c p) f -> p c f", p=128), writes=[R_W3[s]])
            fw.dma(fw.pool, wclk[2][s], W2[s][:], w2[layer, e].rearrange("(c p) d -> p c d", p=128), writes=[R_W2[s]])

        ycnt = [0]

        def H(e, s, st_i, t, hb):
            tok0 = st_i * T + t * 512
            rx = [P.R_xT[(tok0 // 128) + j] for j in range(4)]
            for fc in range(4):
                a, b = pA[fc % 2], pB[fc % 2]
                for dc in range(8):
                    fw.op(fw.pe, nc.tensor.matmul, reads=rx + [R_W1[s]], writes=[R_pA[fc % 2]], out=a[:],
                          lhsT=W1[s][:, dc, fc * 128:(fc + 1) * 128], rhs=P.xT[:, dc, tok0:tok0 + 512], start=(dc == 0), stop=(dc == 7), sig=(dc == 7))
                for dc in range(8):
                    fw.op(fw.pe, nc.tensor.matmul, reads=rx + [R_W3[s]], writes=[R_pB[fc % 2]], out=b[:],
                          lhsT=W3[s][:, dc, fc * 128:(fc + 1) * 128], rhs=P.xT[:, dc, tok0:tok0 + 512], start=(dc == 0), stop=(dc == 7), sig=(dc == 7))
                fw.op(fw.act, nc.scalar.activation, reads=[R_pA[fc % 2]], writes=[R_hs[fc % 2]], out=hs[fc % 2][:], in_=a[:], func=AF.Silu)
                fw.op(fw.dve, nc.vector.tensor_tensor, reads=[R_hs[fc % 2], R_pB[fc % 2]], writes=[R_hp[hb][fc]],
                      out=hp[hb][:, fc, :], in0=hs[fc % 2][:], in1=b[:], op=ALU.mult)

        def Y(e, s, t, hb):
            for tc in range(4):
                c = t * 4 + tc
                for dh in range(2):
                    k = ycnt[0] % 2
                    ycnt[0] += 1
                    for fc in range(4):
                        fw.op(fw.pe, nc.tensor.matmul, reads=[R_hp[hb][fc], R_W2[s]], writes=[R_pY[k]], out=pY[k][:],
                              lhsT=hp[hb][:, fc, tc * 128:(tc + 1) * 128], rhs=W2[s][:, fc, dh * 512:(dh + 1) * 512],
                              start=(fc == 0), stop=(fc == 3), sig=(fc == 3))
                    ysl = yacc[:, c, dh * 512:(dh + 1) * 512]
                    fw.op(fw.dve, nc.vector.scalar_tensor_tensor, reads=[R_pY[k], R_rt, R_y[c][dh]], writes=[R_y[c][dh]],
                          out=ysl, in0=pY[k][:], scalar=gates[:, c, e:e + 1], in1=ysl, op0=ALU.mult, op1=ALU.add)

        for st_i in range(S // T):
            tokS = st_i * T
            load_w(0, 0)
            load_w(1, 1)
            for c in range(NC_T):
                fw.dma(fw.sp, yclk, yacc[:, c, :], src_h[tokS + c * 128: tokS + (c + 1) * 128, :], writes=R_y[c])
            for c in range(NC_T):
                fw.op(fw.act, nc.scalar.mul, reads=R_y[c], writes=R_y[c], out=yacc[:, c, :], in_=yacc[:, c, :], mul=ALPHA)
            for c in range(NC_T):
                blk = tokS // 128 + c
                for dc in range(8):
                    fw.op(fw.pe, nc.tensor.matmul, reads=[P.R_xT[blk], R_wr], writes=[R_pR], out=pR[:, c, :],
                          lhsT=P.xT[:, dc, blk * 128:(blk + 1) * 128], rhs=wr[:, dc, :], start=(dc == 0), stop=(dc == 7), sig=(dc == 7))
            V = nc.vector
            rt = dict(reads=[R_rt], writes=[R_rt])
            fw.op(fw.act, nc.scalar.activation, reads=[R_pR], writes=[R_rt], out=aff[:], in_=pR[:], func=AF.Sigmoid)
            a4 = aff[:].rearrange("p c (g k) -> p c g k", k=4)
            fw.op(fw.dve, V.tensor_tensor, **rt, out=t6[:, :, :, 0:2], in0=a4[:, :, :, 0:2], in1=a4[:, :, :, 2:4], op=ALU.add)
            fw.op(fw.dve, V.tensor_tensor, **rt, out=t6[:, :, :, 2:5], in0=a4[:, :, :, 0:3], in1=a4[:, :, :, 1:4], op=ALU.add)
            fw.op(fw.dve, V.tensor_tensor, **rt, out=t6[:, :, :, 5:6], in0=a4[:, :, :, 0:1], in1=a4[:, :, :, 3:4], op=ALU.add)
            fw.op(fw.dve, V.tensor_reduce, **rt, out=gs[:], in_=t6[:], axis=AX.X, op=ALU.max)
            fw.op(fw.dve, V.tensor_reduce, **rt, out=gmax[:], in_=gs[:], axis=AX.X, op=ALU.max)
            fw.op(fw.dve, V.tensor_tensor, **rt, out=gmask[:], in0=gs[:], in1=gmax[:].unsqueeze(2).to_broadcast([128, NC_T, 4]), op=ALU.is_equal)
            gm_b = gmask[:].unsqueeze(3).to_broadcast([128, NC_T, 4, 4])
            am4 = am[:].rearrange("p c (g k) -> p c g k", k=4)
            fw.op(fw.dve, V.tensor_tensor, **rt, out=am4, in0=a4, in1=gm_b, op=ALU.mult)
            fw.op(fw.dve, V.tensor_tensor, **rt, out=am4, in0=am4, in1=gm_b, op=ALU.add)
            fw.op(fw.dve, V.tensor_scalar_add, **rt, out=am[:], in0=am[:], scalar1=-1.0)
            fw.op(fw.dve, V.tensor_reduce, **rt, out=m1[:], in_=am[:], axis=AX.X, op=ALU.max)
            fw.op(fw.dve, V.tensor_tensor, **rt, out=k1[:], in0=am[:], in1=m1[:].unsqueeze(2).to_broadcast([128, NC_T, NE]), op=ALU.is_equal)
            fw.op(fw.dve, V.scalar_tensor_tensor, **rt, out=am[:], in0=k1[:], scalar=-2.0, in1=am[:], op0=ALU.mult, op1=ALU.add)
            fw.op(fw.dve, V.tensor_reduce, **rt, out=m2[:], in_=am[:], axis=AX.X, op=ALU.max)
            fw.op(fw.dve, V.tensor_tensor, **rt, out=k2[:], in0=am[:], in1=m2[:].unsqueeze(2).to_broadcast([128, NC_T, NE]), op=ALU.is_equal)
            fw.op(fw.dve, V.tensor_tensor, **rt, out=k1[:], in0=k1[:], in1=m1[:].unsqueeze(2).to_broadcast([128, NC_T, NE]), op=ALU.mult)
            fw.op(fw.dve, V.tensor_tensor, **rt, out=k2[:], in0=k2[:], in1=m2[:].unsqueeze(2).to_broadcast([128, NC_T, NE]), op=ALU.mult)
            fw.op(fw.dve, V.tensor_tensor, **rt, out=k1[:], in0=k1[:], in1=k2[:], op=ALU.add)
            fw.op(fw.dve, V.tensor_tensor, **rt, out=m1[:], in0=m1[:], in1=m2[:], op=ALU.add)
            fw.op(fw.dve, V.reciprocal, **rt, out=m1[:], in_=m1[:])
            fw.op(fw.dve, V.tensor_tensor, **rt, out=gates[:], in0=k1[:], in1=m1[:].unsqueeze(2).to_broadcast([128, NC_T, NE]), op=ALU.mult)
            prev = None
            hb = 0
            for e in range(NE):
                s = e % 2
                for t in range(NT5):
                    H(e, s, st_i, t, hb)
                    if prev is not None:
                        Y(*prev)
                        pe_, ps_, pt_, phb_ = prev
                        if pt_ == NT5 - 1 and pe_ + 2 < NE:
                            load_w(pe_ + 2, ps_)
                    prev = (e, s, t, hb)
                    hb ^= 1
            Y(*prev)
            for c in range(NC_T):
                blk = tokS // 128 + c
                z = yacc[:, c, :]
                _ln_two_regs(P, z, R_y[c], lng, lnb, R_lnp)
                fw.dma(fw.sp, yclk, dst_h[tokS + c * 128: tokS + (c + 1) * 128, :], z, reads=R_y[c])
                if make_xT:
                    _to_xT_two(P, z, R_y[c], blk)
        P.phase_barrier()


def _ln_two_regs(P, z, Rs, lng, lnb, R_lnp, on_dve=False, pow_on_pool=False):
    nc, fw = P.nc, P.fw
    k = P.ln_ctr % 2
    P.ln_ctr += 1
    st, mv, rstd, R = P.ln_stats[k], P.ln_mv[k], P.ln_rstd[k], P.R_ln[k]
    for i in range(2):
        fw.op(fw.dve, nc.vector.bn_stats, reads=Rs, writes=[R], out=st[:, i, :], in_=z[:, i * 512:(i + 1) * 512])
    fw.op(fw.dve, nc.vector.bn_aggr, reads=[R], writes=[R], out=mv[:], in_=st[:].rearrange("p a b -> p (a b)"))
    fw.op(fw.dve, nc.vector.tensor_scalar_add, reads=[R], writes=[R], out=rstd[:], in0=mv[:, 1:2], scalar1=LN_EPS)
    if on_dve and not pow_on_pool:
        fw.op(fw.act, nc.scalar.activation, reads=[R], writes=[R], out=rstd[:], in_=rstd[:], func=AF.Sqrt)
        fw.op(fw.dve, nc.vector.reciprocal, reads=[R], writes=[R], out=rstd[:], in_=rstd[:])
    else:
        fw.op(fw.pool, nc.gpsimd.tensor_tensor, reads=[R, P.R_const], writes=[R], out=rstd[:], in0=rstd[:], in1=P.mhalf[:], op=ALU.pow)
    fw.op(fw.dve, nc.vector.tensor_scalar, reads=list(Rs) + [R], writes=Rs, out=z, in0=z, scalar1=mv[:, 0:1], scalar2=rstd[:],
          op0=ALU.subtract, op1=ALU.mult)
    AE, AI = (fw.dve, nc.vector) if on_dve else (fw.pool, nc.gpsimd)
    fw.op(AE, AI.tensor_tensor, reads=list(Rs) + [R_lnp], writes=Rs, out=z, in0=z, in1=lng[:], op=ALU.mult)
    fw.op(AE, AI.tensor_tensor, reads=list(Rs) + [R_lnp], writes=Rs, out=z, in0=z, in1=lnb[:], op=ALU.add)


def _ln_act(P, z, Rz, lng, lnb, R_lnp, T):
    nc, fw = P.nc, P.fw
    k = T["ctr"][0] % 2
    T["ctr"][0] += 1
    junk, R_junk = T["junk"], T["R_junk"]
    s, R = T["s"][k], T["R"][k]
    A_, V_ = nc.scalar, nc.vector
    fw.op(fw.act, A_.activation, reads=[Rz], writes=[R_junk, R], out=junk[:], in_=z, func=AF.Identity, accum_out=s[:, 0:1])
    fw.op(fw.act, A_.activation, reads=[Rz], writes=[R_junk, R], out=junk[:], in_=z, func=AF.Square, accum_out=s[:, 1:2])
    rr = dict(reads=[R], writes=[R])
    fw.op(fw.dve, V_.tensor_scalar_mul, **rr, out=s[:, 2:3], in0=s[:, 0:1], scalar1=1.0 / D)
    fw.op(fw.dve, V_.tensor_tensor, **rr, out=s[:, 3:4], in0=s[:, 2:3], in1=s[:, 2:3], op=ALU.mult)
    fw.op(fw.dve, V_.scalar_tensor_tensor, **rr, out=s[:, 4:5], in0=s[:, 1:2], scalar=1.0 / D, in1=s[:, 3:4], op0=ALU.mult, op1=ALU.subtract)
    fw.op(fw.dve, V_.tensor_scalar_add, **rr, out=s[:, 4:5], in0=s[:, 4:5], scalar1=LN_EPS)
    fw.op(fw.act, A_.activation, **rr, out=s[:, 5:6], in_=s[:, 4:5], func=AF.Sqrt)
    fw.op(fw.dve, V_.reciprocal, **rr, out=s[:, 5:6], in_=s[:, 5:6])
    fw.op(fw.dve, V_.scalar_tensor_tensor, **rr, out=s[:, 6:7], in0=s[:, 2:3], scalar=-1.0, in1=s[:, 5:6], op0=ALU.mult, op1=ALU.mult)
    fw.op(fw.act, A_.activation, reads=[Rz, R], writes=[Rz], out=z, in_=z, func=AF.Identity, scale=s[:, 5:6], bias=s[:, 6:7])
    fw.op(fw.dve, V_.tensor_tensor, reads=[Rz, R_lnp], writes=[Rz], out=z, in0=z, in1=lng[:], op=ALU.mult)
    fw.op(fw.dve, V_.tensor_tensor, reads=[Rz, R_lnp], writes=[Rz], out=z, in0=z, in1=lnb[:], op=ALU.add)


def _to_xT_two(P, z, Rs, blk):
    nc, fw = P.nc, P.fw
    k = blk % 2
    zb, R_zb = P.zb[k], P.R_zb[k]
    fw.op(fw.act, nc.scalar.copy, reads=Rs, writes=[R_zb], out=zb[:], in_=z)
    for c in range(8):
        fw.op(fw.pe, nc.tensor.transpose, reads=[R_zb, P.R_const], writes=[P.R_pTR], out=P.pTR[:, c, :],
              in_=zb[:, c * 128:(c + 1) * 128], identity=P.identb[:])
    fw.op(fw.dve, nc.vector.tensor_copy, reads=[P.R_pTR], writes=[P.R_xT[blk]], out=P.xT[:, :, blk * 128:(blk + 1) * 128], in_=P.pTR[:])


def _phase_barrier(self):
    fw = self.fw
    clocks = [e.clock for e in (fw.pe, fw.act, fw.dve, fw.pool, fw.sp)] + list(fw._dmaclk.values())
    for E in (fw.pe, fw.act, fw.dve, fw.pool, fw.sp):
        fw.wait_all(E, [c for c in clocks if c is not E.clock])


Prog.phase_barrier = _phase_barrier


def residual_ln_out(P, st_tiles, mix_psum, R_mix, x_src, dst_h, blk, ln_idx, lng, lnb, R_lnp, defer_cast=False, stage=0):
    nc, fw = P.nc, P.fw
    k = blk % 2
    z, R_z, clk = st_tiles["z"][k], st_tiles["R_z"][k], st_tiles["zclk"][k]
    if stage in (0, 1):
        fw.dma(fw.sp, clk, z[:], x_src[blk * 128:(blk + 1) * 128, :], writes=[R_z])
        fw.op(fw.dve, nc.vector.scalar_tensor_tensor, reads=[R_z, R_mix], writes=[R_z], out=z[:], in0=z[:], scalar=ALPHA, in1=mix_psum,
              op0=ALU.mult, op1=ALU.add)
    if stage == 1:
        return
    kk = P.ln_ctr % 2
    P.ln_ctr += 1
    st, mv, rstd, R = P.ln_stats[kk], P.ln_mv[kk], P.ln_rstd[kk], P.R_ln[kk]
    for i in range(2):
        fw.op(fw.dve, nc.vector.bn_stats, reads=[R_z], writes=[R], out=st[:, i, :], in_=z[:, i * 512:(i + 1) * 512])
    fw.op(fw.dve, nc.vector.bn_aggr, reads=[R], writes=[R], out=mv[:], in_=st[:].rearrange("p a b -> p (a b)"))
    fw.op(fw.dve, nc.vector.tensor_scalar_add, reads=[R], writes=[R], out=rstd[:], in0=mv[:, 1:2], scalar1=LN_EPS)
    fw.op(fw.pool, nc.gpsimd.tensor_tensor, reads=[R, P.R_const], writes=[R], out=rstd[:], in0=rstd[:], in1=P.mhalf[:], op=ALU.pow)
    fw.op(fw.dve, nc.vector.tensor_scalar, reads=[R_z, R], writes=[R_z], out=z[:], in0=z[:], scalar1=mv[:, 0:1], scalar2=rstd[:],
          op0=ALU.subtract, op1=ALU.mult)
    fw.op(fw.pool, nc.gpsimd.tensor_tensor, reads=[R_z, R_lnp], writes=[R_z], out=z[:], in0=z[:], in1=lng[:], op=ALU.mult)
    fw.op(fw.pool, nc.gpsimd.tensor_tensor, reads=[R_z, R_lnp], writes=[R_z], out=z[:], in0=z[:], in1=lnb[:], op=ALU.add)
    fw.dma(fw.sp, clk, dst_h[blk * 128:(blk + 1) * 128, :], z[:], reads=[R_z])
    if not defer_cast:
        zb_cast(P, z, R_z, blk)


def zb_cast(P, z, R_z, blk):
    P.fw.op(P.fw.act, P.nc.scalar.copy, reads=[R_z], writes=[P.R_zb[blk % 2]], out=P.zb[blk % 2][:], in_=z[:])


def xT_finish(P, blk):
    nc, fw = P.nc, P.fw
    k = blk % 2
    for c in range(8):
        fw.op(fw.pe, nc.tensor.transpose, reads=[P.R_zb[k], P.R_const], writes=[P.R_pTR], out=P.pTR[:, c, :],
              in_=P.zb[k][:, c * 128:(c + 1) * 128], identity=P.identb[:])
    fw.op(fw.dve, nc.vector.tensor_copy, reads=[P.R_pTR], writes=[P.R_xT[blk]], out=P.xT[:, :, blk * 128:(blk + 1) * 128], in_=P.pTR[:])


def zb_double(P, st):
    zb1 = P.sb("zb1", [128, D], BF16, st)
    old = (P.zb, P.R_zb)
    P.zb = [P.zb[0], zb1]
    P.R_zb = [P.R_zb[0], P.reg("zb1")]
    return old


def mm_group(P, out, pairs, reads, writes):
    nc, fw = P.nc, P.fw
    n = len(pairs)
    for i, (l, r) in enumerate(pairs):
        fw.op(fw.pe, nc.tensor.matmul, reads=reads, writes=writes, out=out, lhsT=l, rhs=r, start=(i == 0), stop=(i == n - 1), sig=(i == n - 1))


def gla_phase(P, x_src, dst_h, ln_idx, p0_src=None):
    nc, fw = P.nc, P.fw
    S, NB = P.S, P.NB
    H, DK, DV = 4, 128, 256
    V, A, G_ = nc.vector, nc.scalar, nc.gpsimd
    w_in = P.dram["gla_w_in"]
    with ExitStack() as st:
        sb = lambda n, s, d: P.sb(n, s, d, st)
        Win = sb("gWin", [128, 8, 3088], BF16)
        Wout = sb("gWout", [128, 8, D], BF16)
        Wgu = sb("gWgu", [16, 512], BF16)
        negb = sb("g_negb", [128, 4], F32)
        ngh = sb("g_ngh", [128, D], F32)
        lng = sb("g_lng", [128, D], F32)
        lnb = sb("g_lnb", [128, D], F32)
        tri = sb("g_tri", [128, 128], F32)
        rmask = sb("g_rmask", [128, 512], F32)
        R_w = P.reg("gw")
        R_lnp = P.reg("glnp")
        wclk = fw.dma_clock("gla_w")
        for dc in range(8):
            fw.dma(fw.pool, wclk, Win[:, dc, :], w_in[0, dc * 128:(dc + 1) * 128, :], writes=[R_w])
        fw.dma(fw.pool, wclk, Wgu[:], P.dram["gla_w_gate_up"][0], writes=[R_w])
        R_wout = P.reg("gwout")
        fw.dma(fw.pool, fw.dma_clock("gla_wout"), Wout[:], P.dram["gla_w_out"][0].rearrange("(c p) d -> p c d", p=128), writes=[R_wout])
        fw.dma(fw.sp, wclk, negb[:], P.dram["l_gla_b_gate"], writes=[R_w])
        fw.dma(fw.sp, wclk, ngh[:], P.dram["gla_norm_g"][0, :].partition_broadcast(128), writes=[R_w])
        fw.dma(fw.sp, wclk, tri[:], P.dram["c_tri"], writes=[R_w])
        fw.dma(fw.sp, wclk, rmask[:], P.dram["c_rmask"], writes=[R_w])
        fw.dma(fw.sp, wclk, lng[:], P.dram["ln_g"][ln_idx // 2, ln_idx % 2, :].partition_broadcast(128), writes=[R_lnp])
        fw.dma(fw.sp, wclk, lnb[:], P.dram["ln_b"][ln_idx // 2, ln_idx % 2, :].partition_broadcast(128), writes=[R_lnp])
        fw.op(fw.act, A.mul, reads=[R_w], writes=[R_w], out=negb[:], in_=negb[:], mul=-1.0)
        fw.op(fw.act, A.mul, reads=[R_w], writes=[R_w], out=ngh[:], in_=ngh[:], mul=0.5)
        S32 = sb("gS32", [128, H, DV], F32)
        S16 = sb("gS16", [128, H, DV], BF16)
        R_S32, R_S16 = P.regs(H, "S32_"), P.regs(H, "S16_")
        for h in range(H):
            fw.op(fw.pool, G_.memset, writes=[R_S32[h]], ap=S32[:, h, :], constant=0.0)
            fw.op(fw.pool, G_.memset, writes=[R_S16[h]], ap=S16[:, h, :], constant=0.0)
        glr = sb("g_glr", [16, 512], BF16); R_glr = P.reg()
        e1 = sb("g_e1", [128, 512], F32); R_e1 = P.reg()
        cs = sb("g_cs", [128, 512], F32); R_cs = P.reg()
        Eb = sb("g_Eb", [128, H, 512], F32); R_Eb = P.regs(H)
        En = sb("g_En", [128, 512], F32); R_En = P.reg()
        qe = sb("g_qe", [128, H, 512], BF16); R_qe = P.regs(H)
        kn32 = sb("g_kn32", [128, H, 512], F32); R_kn32 = P.regs(H)
        kneg = sb("g_kneg", [128, H, 512], BF16); R_kneg = P.regs(H)
        v_sb = sb("g_v", [128, D], BF16); R_v = P.reg()
        th = sb("g_th", [128, D], F32); R_th = P.reg()
        kd = sb("g_kd", [128, 128], BF16); R_kd = P.reg()
        kdT = sb("g_kdT", [128, 128], BF16); R_kdT = P.reg()
        scT = sb("g_scT", [128, 128], BF16); R_scT = P.reg()
        on = sb("g_on", [128, D], F32); R_on = P.reg()
        og = [sb(f"g_og{i}", [128, D], BF16) for i in range(2)]; R_og = P.regs(2)
        ogT = sb("g_ogT", [128, 8, 128], BF16); R_ogT = P.reg()
        hst = sb("g_hst", [128, H, 6], F32)
        hmv = sb("g_hmv", [128, H, 2], F32)
        hrs = sb("g_hrs", [128, H], F32); R_hn = P.reg()
        _z = sb("g_z", [128, D], F32); _Rz = P.reg()
        tiles = {"z": [_z, _z], "R_z": [_Rz, _Rz], "zclk": [fw.dma_clock("gz")] * 2}
        if p0_src is not None:
            p0clk = fw.dma_clock("p0")
            for bb in range(NB):
                fw.dma(fw.sp, p0clk, on[:], p0_src[bb * 128:(bb + 1) * 128, :], writes=[R_on])
                _to_xT_two(P, on[:], [R_on], bb)
        pFM = [P.ps(f"g_pFM{i}", [128, 512], F32, st) for i in range(2)]; R_pFM = P.regs(2)
        pTM = [P.ps(f"g_pTM{i}", [128, D], F32, st) for i in range(2)]; R_pTM = P.regs(2)
        pSC = P.ps("g_pSC", [128, 512], F32, st); R_pSC = P.reg()
        fmc = [0]

        def fm():
            k = fmc[0] % 2
            fmc[0] += 1
            return pFM[k], R_pFM[k]

        def group_stage(g):
            tok0 = g * 512
            rx = [P.R_xT[4 * g + j] for j in range(4)]
            xg = lambda dc: P.xT[:, dc, tok0:tok0 + 512]
            p, Rp = fm()
            mm_group(P, p[0:16, :], [(Win[:, dc, 3072:3088], xg(dc)) for dc in range(8)], rx + [R_w], [Rp])
            fw.op(fw.act, A.copy, reads=[Rp], writes=[R_glr], out=glr[:], in_=p[0:16, :])
            for h in range(H):
                p, Rp = fm()
                mm_group(P, p[:], [(Wgu[0:16, h * 128:(h + 1) * 128], glr[0:16, :])], [R_glr, R_w], [Rp])
                fw.op(fw.act, A.activation, reads=[Rp, R_w], writes=[R_e1], out=e1[:], in_=p[:], func=AF.Exp, scale=-1.0, bias=negb[:, h:h + 1])
                fw.op(fw.act, A.activation, reads=[R_e1], writes=[R_e1], out=e1[:], in_=e1[:], func=AF.Ln, bias=1.0)
                fw.op(fw.dve, V.tensor_tensor_scan, reads=[R_e1, R_w], writes=[R_cs], out=cs[:], data0=rmask[:], data1=e1[:], initial=0.0,
                      op0=ALU.mult, op1=ALU.add)
                fw.op(fw.act, A.activation, reads=[R_cs], writes=[R_Eb[h]], out=Eb[:, h, :], in_=cs[:], func=AF.Exp, scale=-1.0 / 16.0)
                fw.op(fw.act, A.activation, reads=[R_cs], writes=[R_En], out=En[:], in_=cs[:], func=AF.Exp, scale=1.0 / 16.0)
                p, Rp = fm()
                mm_group(P, p[:], [(Win[:, dc, h * 128:(h + 1) * 128], xg(dc)) for dc in range(8)], rx + [R_w], [Rp])
                fw.op(fw.dve, V.scalar_tensor_tensor, reads=[Rp, R_Eb[h]], writes=[R_qe[h]], out=qe[:, h, :], in0=p[:], scalar=DK ** -0.5,
                      in1=Eb[:, h, :], op0=ALU.mult, op1=ALU.mult)
                p, Rp = fm()
                mm_group(P, p[:], [(Win[:, dc, 512 + h * 128:512 + (h + 1) * 128], xg(dc)) for dc in range(8)], rx + [R_w], [Rp])
                fw.op(fw.dve, V.tensor_tensor, reads=[Rp, R_En], writes=[R_kn32[h]], out=kn32[:, h, :], in0=p[:], in1=En[:], op=ALU.mult)
                fw.op(fw.act, A.copy, reads=[R_kn32[h]], writes=[R_kneg[h]], out=kneg[:, h, :], in_=kn32[:, h, :])
        def front(blk):
            if True:
                g, j = divmod(blk, 4)
                if j == 0:
                    group_stage(g)
                tok0 = g * 512
                ogb, R_ogb = og[blk % 2], R_og[blk % 2]
                ts = slice(tok0 + j * 128, tok0 + (j + 1) * 128)
                ls = slice(j * 128, (j + 1) * 128)
                rxb = [P.R_xT[blk], R_w]
                for half in range(2):
                    mm_group(P, pTM[0][:, half * 512:(half + 1) * 512],
                             [(P.xT[:, dc, ts], Win[:, dc, 1024 + half * 512:1024 + (half + 1) * 512]) for dc in range(8)], rxb, [R_pTM[0]])
                fw.op(fw.act, A.copy, reads=[R_pTM[0]], writes=[R_v], out=v_sb[:], in_=pTM[0][:])
                for half in range(2):
                    mm_group(P, pTM[1][:, half * 512:(half + 1) * 512],
                             [(P.xT[:, dc, ts], Win[:, dc, 2048 + half * 512:2048 + (half + 1) * 512]) for dc in range(8)], rxb, [R_pTM[1]])
                fw.op(fw.act, A.activation, reads=[R_pTM[1]], writes=[R_th], out=th[:], in_=pTM[1][:], func=AF.Tanh, scale=0.5)
                fw.op(fw.dve, V.scalar_tensor_tensor, reads=[R_th, R_pTM[1]], writes=[R_th], out=th[:], in0=th[:], scalar=1.0, in1=pTM[1][:],
                      op0=ALU.add, op1=ALU.mult)
                fw.op(fw.pool, G_.tensor_tensor, reads=[R_th, R_w], writes=[R_th], out=th[:], in0=th[:], in1=ngh[:], op=ALU.mult)

        def front_B(blk):
            if True:
                g, j = divmod(blk, 4)
                tok0 = g * 512
                ogb, R_ogb = og[blk % 2], R_og[blk % 2]
                ls = slice(j * 128, (j + 1) * 128)
                for h in range(H):
                    elast = Eb[:, h, j * 128 + 127:j * 128 + 128]
                    hs_ = slice(h * DV, (h + 1) * DV)
                    fw.op(fw.act, A.activation, reads=[R_kn32[h], R_Eb[h]], writes=[R_kd], out=kd[:], in_=kn32[:, h, ls], func=AF.Identity, scale=elast)
                    fw.op(fw.pe, nc.tensor.transpose, reads=[R_kd, P.R_const], writes=[P.R_pTR], out=P.pTR[:, 0, :], in_=kd[:], identity=P.identb[:])
                    fw.op(fw.dve, V.tensor_copy, reads=[P.R_pTR], writes=[R_kdT], out=kdT[:], in_=P.pTR[:, 0, :])
                    mm_group(P, pSC[:, 0:128], [(kneg[:, h, ls], qe[:, h, ls])], [R_kneg[h], R_qe[h]], [R_pSC])
                    fw.op(fw.dve, V.tensor_tensor, reads=[R_pSC, R_w], writes=[R_scT], out=scT[:], in0=pSC[:, 0:128], in1=tri[:], op=ALU.mult)
                    mm_group(P, pTM[0][:, hs_], [(scT[:], v_sb[:, hs_]), (qe[:, h, ls], S16[:, h, :])], [R_scT, R_v, R_qe[h], R_S16[h]], [R_pTM[0]])
                    mm_group(P, pSC[:, 128:384], [(kdT[:], v_sb[:, hs_])], [R_kdT, R_v], [R_pSC])
                    fw.op(fw.dve, V.scalar_tensor_tensor, reads=[R_S32[h], R_Eb[h], R_pSC], writes=[R_S32[h]], out=S32[:, h, :], in0=S32[:, h, :],
                          scalar=elast, in1=pSC[:, 128:384], op0=ALU.mult, op1=ALU.add)
                    fw.op(fw.pool, G_.tensor_copy, reads=[R_S32[h]], writes=[R_S16[h]], out=S16[:, h, :], in_=S32[:, h, :])
                fw.op(fw.act, A.copy, reads=[R_pTM[0]], writes=[R_on], out=on[:], in_=pTM[0][:])
                for h in range(H):
                    fw.op(fw.dve, V.bn_stats, reads=[R_on], writes=[R_hn], out=hst[:, h, :], in_=on[:, h * DV:(h + 1) * DV])
                for h in range(H):
                    fw.op(fw.dve, V.bn_aggr, reads=[R_hn], writes=[R_hn], out=hmv[:, h, :], in_=hst[:, h, :])
                fw.op(fw.dve, V.tensor_scalar_add, reads=[R_hn], writes=[R_hn], out=hrs[:], in0=hmv[:, :, 1], scalar1=LN_EPS)
                fw.op(fw.pool, G_.tensor_tensor, reads=[R_hn, P.R_const], writes=[R_hn], out=hrs[:], in0=hrs[:], in1=P.mhalf[:].to_broadcast([128, H]), op=ALU.pow)
                for h in range(H):
                    fw.op(fw.dve, V.tensor_scalar, reads=[R_on, R_hn], writes=[R_on], out=on[:, h * DV:(h + 1) * DV], in0=on[:, h * DV:(h + 1) * DV],
                          scalar1=hmv[:, h, 0:1], scalar2=hrs[:, h:h + 1], op0=ALU.subtract, op1=ALU.mult)
                fw.op(fw.dve, V.tensor_tensor, reads=[R_on, R_th], writes=[R_ogb], out=ogb[:], in0=on[:], in1=th[:], op=ALU.mult)

        def tailA(blk):
            if True:
                ogb, R_ogb = og[blk % 2], R_og[blk % 2]
                for c in range(8):
                    fw.op(fw.pe, nc.tensor.transpose, reads=[R_ogb, P.R_const], writes=[P.R_pTR], out=P.pTR[:, c, :], in_=ogb[:, c * 128:(c + 1) * 128],
                          identity=P.identb[:])
                fw.op(fw.act, A.copy, reads=[P.R_pTR], writes=[R_ogT], out=ogT[:], in_=P.pTR[:])
                for half in range(2):
                    mm_group(P, pTM[1][:, half * 512:(half + 1) * 512],
                             [(ogT[:, vc, :], Wout[:, vc, half * 512:(half + 1) * 512]) for vc in range(8)], [R_ogT, R_wout], [R_pTM[1]])
                residual_ln_out(P, tiles, pTM[1][:], R_pTM[1], x_src, dst_h, blk, ln_idx, lng, lnb, R_lnp, defer_cast=True)

        for b in range(NB + 1):
            if b < NB:
                front(b)
            if 0 <= b - 1 < NB:
                tailA(b - 1)
            if b < NB:
                front_B(b)
            if 0 <= b - 1 < NB:
                zb_cast(P, _z, _Rz, b - 1)
                xT_finish(P, b - 1)
        P.phase_barrier()


def mlstm_phase(P, x_src, dst_h, ln_idx):
    nc, fw = P.nc, P.fw
    S, NB = P.S, P.NB
    H, DH = 4, 256
    GRP = 256
    V, A, G_ = nc.vector, nc.scalar, nc.gpsimd
    w_in = P.dram["mlstm_w_in"]
    a_dram = P.dscr("a_dram", [4, S])
    NEG = -80.0
    with ExitStack() as st:
        sb = lambda n, s, d: P.sb(n, s, d, st)
        cols = sb("m_cols", [128, NB, 16], F32); R_cols = P.reg()
        woldbc = sb("m_woldbc", [128, H, NB], F32); R_wold = P.reg()
        Wg = sb("m_Wg", [128, 8, 8], BF16)
        bg = sb("m_bg", [4, 2], F32)
        nbf = sb("m_nbf", [4, 1], F32)
        tri = sb("m_tri", [128, 128], F32)
        cwh = sb("m_cwh", [128, 16, 4], F32)
        cbh = sb("m_cbh", [128, 16], F32)
        ngc = sb("m_ngc", [128, 8], F32)
        onesb = sb("m_onesb", [128, 1], BF16)
        R_w = P.reg("mw"); R_lnp = P.reg("mlnp")
        wclk = fw.dma_clock("ml_w")
        fw.dma(fw.pool, wclk, Wg[:], w_in[0, :, 4096:4104].rearrange("(c p) f -> p c f", p=128), writes=[R_w])
        for (t_, name) in [(bg, "l_mb"), (tri, "c_trineg"), (cwh, "l_conv_w"), (cbh, "l_conv_b"), (ngc, "l_mnormg")]:
            fw.dma(fw.sp, wclk, t_[:], P.dram[name], writes=[R_w])
        fw.op(fw.pool, G_.memset, writes=[R_w], ap=onesb[:], constant=1.0)
        fw.op(fw.act, A.mul, reads=[R_w], writes=[R_w], out=nbf[:], in_=bg[:, 1:2], mul=-1.0)
        fw.op(fw.act, A.mul, reads=[R_w], writes=[R_w], out=ngc[:], in_=ngc[:], mul=0.5)
        pFM = [P.ps(f"m_pFM{i}", [128, 512], F32, st) for i in range(2)]; R_pFM = P.regs(2)
        pTM = P.ps("m_pTM", [128, D], F32, st); R_pTM = P.reg()
        pSC = P.ps("m_pSC", [128, 512], F32, st); R_pSC = P.reg()
        pSQ = P.ps("m_pSQ", [128, 512], F32, st); R_pSQ = P.reg()
        pCU = P.ps("m_pCU", [128, 2, 256], F32, st); R_pCU = P.reg()
        fmc = [0]

        def fm():
            k = fmc[0] % 2
            fmc[0] += 1
            return pFM[k], R_pFM[k]

        Win = sb("mWin", [128, 8, 4096], BF16)
        R_win = P.reg("mwin")
        for dc in range(8):
            fw.dma(fw.pool, fw.dma_clock("ml_win"), Win[:, dc, :], w_in[0, dc * 128:(dc + 1) * 128, 0:4096], writes=[R_win])
        with ExitStack() as st2:
            T1 = P.sb("m_T1", [4, S], F32, st2); T2 = P.sb("m_T2", [4, S], F32, st2)
            T3 = P.sb("m_T3", [4, S], F32, st2); T4 = P.sb("m_T4", [4, S], F32, st2)
            sel = P.sb("m_sel", [4, H, 128], F32, st2)
            ones4 = P.sb("m_ones4", [4, 128], F32, st2)
            fw.dma(fw.sp, wclk, sel[:], P.dram["c_sel"], writes=[R_w])
            fw.op(fw.pool, G_.memset, writes=[R_w], ap=ones4[:], constant=1.0)
            mnew = P.sb("m_mnew", [4, NB], F32, st2); mprev = P.sb("m_mprev", [4, NB], F32, st2)
            blast = P.sb("m_blast", [4, NB], F32, st2); dlt = P.sb("m_dlt", [4, NB], F32, st2); wold = P.sb("m_wold", [4, NB], F32, st2)
            Rr = P.reg("rows")
            rr = dict(reads=[Rr], writes=[Rr])
            for t5 in range(S // 512):
                tsl = slice(t5 * 512, (t5 + 1) * 512)
                rx = [P.R_xT[4 * t5 + j] for j in range(4)]
                p, Rp = fm()
                mm_group(P, p[0:4, :], [(Wg[:, dc, 0:4], P.xT[:, dc, tsl]) for dc in range(8)], rx + [R_w], [Rp])
                fw.op(fw.act, A.activation, reads=[Rp, R_w], writes=[Rr], out=T1[:, tsl], in_=p[0:4, :], func=AF.Identity, bias=bg[:, 0:1])
                p, Rp = fm()
                mm_group(P, p[0:4, :], [(Wg[:, dc, 4:8], P.xT[:, dc, tsl]) for dc in range(8)], rx + [R_w], [Rp])
                fw.op(fw.act, A.activation, reads=[Rp, R_w], writes=[Rr], out=T2[:, tsl], in_=p[0:4, :], func=AF.Exp, scale=-1.0, bias=nbf[:])
            fw.op(fw.act, A.activation, **rr, out=T2[:], in_=T2[:], func=AF.Ln, bias=1.0)
            fw.op(fw.act, A.mul, **rr, out=T2[:], in_=T2[:], mul=-1.0)
            for b in range(NB):
                bs = slice(b * 128, (b + 1) * 128)
                fw.op(fw.dve, V.tensor_tensor_scan, reads=[Rr, R_w], writes=[Rr], out=T3[:, bs], data0=ones4[:], data1=T2[:, bs], initial=0.0,
                      op0=ALU.mult, op1=ALU.add)
            fw.op(fw.dve, V.tensor_tensor_scan, **rr, out=T4[:], data0=T2[:], data1=T1[:], initial=-1e30, op0=ALU.add, op1=ALU.max)
            last = lambda T: T[:].rearrange("h (b t) -> h b t", t=128)[:, :, 127]
            fw.op(fw.dve, V.tensor_copy, **rr, out=mnew[:], in_=last(T4))
            fw.op(fw.dve, V.tensor_copy, **rr, out=blast[:], in_=last(T3))
            fw.op(fw.dve, V.memset, **rr, ap=mprev[:, 0:1], constant=-1e30)
            if NB > 1:
                fw.op(fw.dve, V.tensor_copy, **rr, out=mprev[:, 1:NB], in_=mnew[:, 0:NB - 1])
            fw.op(fw.dve, V.tensor_tensor, **rr, out=T2[:], in0=T3[:], in1=T4[:], op=ALU.subtract)
            fw.dma(fw.sp, fw.dma_clock("a_out"), a_dram, T2[:], reads=[Rr], writes=[R_cols])
            fw.op(fw.dve, V.tensor_tensor, **rr, out=T1[:], in0=T1[:], in1=T3[:], op=ALU.subtract)
            v3 = lambda T: T[:].rearrange("h (b t) -> h b t", t=128)
            bc = lambda t_: t_[:].unsqueeze(2).to_broadcast([4, NB, 128])
            fw.op(fw.dve, V.tensor_tensor, **rr, out=v3(T2), in0=v3(T2), in1=bc(mprev), op=ALU.add)
            fw.op(fw.dve, V.tensor_scalar_max, **rr, out=T2[:], in0=T2[:], scalar1=NEG)
            fw.op(fw.act, A.activation, **rr, out=T2[:], in_=T2[:], func=AF.Exp)
            fw.op(fw.act, A.activation, **rr, out=T4[:], in_=T4[:], func=AF.Exp, scale=-1.0)
            fw.op(fw.dve, V.tensor_tensor, **rr, out=dlt[:], in0=blast[:], in1=mnew[:], op=ALU.subtract)
            fw.op(fw.dve, V.tensor_tensor, **rr, out=wold[:], in0=dlt[:], in1=mprev[:], op=ALU.add)
            fw.op(fw.dve, V.tensor_scalar_max, **rr, out=wold[:], in0=wold[:], scalar1=NEG)
            fw.op(fw.act, A.activation, **rr, out=wold[:], in_=wold[:], func=AF.Exp)
            fw.op(fw.dve, V.tensor_scalar_add, **rr, out=dlt[:], in0=dlt[:], scalar1=-float(np.log(16.0)))
            fw.op(fw.dve, V.tensor_tensor, **rr, out=v3(T3), in0=v3(T1), in1=bc(dlt), op=ALU.add)
            fw.op(fw.act, A.activation, **rr, out=T3[:], in_=T3[:], func=AF.Exp)
            pcol = pSQ[:].rearrange("p (b q) -> p b q", q=16)
            for b in range(NB):
                bs = slice(b * 128, (b + 1) * 128)
                for qi, T in enumerate([T1, T2, T4, T3]):
                    fw.op(fw.pe, nc.tensor.transpose, reads=[Rr, P.R_const], writes=[R_pSQ], out=pcol[:, b, 4 * qi:4 * qi + 4], in_=T[0:4, bs],
                          identity=P.identf[0:4, 0:4])
            fw.op(fw.dve, V.tensor_copy, reads=[R_pSQ], writes=[R_cols], out=cols[:], in_=pcol[:, 0:NB, :])
            for h in range(H):
                mm_group(P, pSC[:, 0:NB], [(sel[0:4, h, :], wold[0:4, :])], [Rr, R_w], [R_pSC])
                fw.op(fw.dve, V.tensor_copy, reads=[R_pSC], writes=[R_wold], out=woldbc[:, h, :], in_=pSC[:, 0:NB])
            P.phase_barrier()
        Wout = sb("mWout", [128, 8, D], BF16)
        lng = sb("m_lng", [128, D], F32)
        lnb = sb("m_lnb", [128, D], F32)
        fw.dma(fw.sp, wclk, lng[:], P.dram["ln_g"][ln_idx // 2, ln_idx % 2, :].partition_broadcast(128), writes=[R_lnp])
        fw.dma(fw.sp, wclk, lnb[:], P.dram["ln_b"][ln_idx // 2, ln_idx % 2, :].partition_broadcast(128), writes=[R_lnp])
        z = sb("m_z", [128, D], F32); R_z = P.reg()
        tiles = {"z": [z, z], "R_z": [R_z, R_z], "zclk": [fw.dma_clock("mz")] * 2}
        for vc in range(8):
            fw.dma(fw.sp, tiles["zclk"][0], z[:], P.dram["mlstm_w_out"][0, vc * 128:(vc + 1) * 128, :], writes=[R_z])
            fw.op(fw.act, A.activation, reads=[R_z, R_w], writes=[R_w], out=Wout[:, vc, :], in_=z[:], func=AF.Identity, scale=ngc[:, vc:vc + 1])
        C32 = sb("mC32", [128, H, 2, DH], F32); C16 = sb("mC16", [128, H, 2, DH], BF16)
        n32 = sb("mn32", [128, H, 2], F32); n16 = sb("mn16", [128, H, 2], BF16)
        R_C32, R_C16 = P.regs(H), P.regs(H)
        halo = sb("m_halo", [128, 16, 3], F32); R_halo = P.regs(16)
        for h in range(H):
            fw.op(fw.pool, G_.memset, writes=[R_C32[h]], ap=C32[:, h], constant=0.0)
            fw.op(fw.pool, G_.memset, writes=[R_C16[h]], ap=C16[:, h], constant=0.0)
            fw.op(fw.pool, G_.memset, writes=[R_C32[h]], ap=n32[:, h, :], constant=0.0)
            fw.op(fw.pool, G_.memset, writes=[R_C16[h]], ap=n16[:, h, :], constant=0.0)
        for ch in range(16):
            fw.op(fw.pool, G_.memset, writes=[R_halo[ch]], ap=halo[:, ch, :], constant=0.0)
        qk = sb("m_qk", [128, 16, GRP], BF16); R_qk = P.regs(16)
        pre2 = [sb(f"m_pre{i}", [128, GRP + 4], F32) for i in range(2)]; R_pre2 = P.regs(2)
        acc2 = [sb(f"m_acc{i}", [128, GRP], F32) for i in range(2)]; R_acc2 = P.regs(2)
        v_sb = sb("m_v", [128, D], BF16); R_v = P.reg()
        og = [sb(f"m_og{i}", [128, D], BF16) for i in range(2)]; R_og = P.regs(2)
        tho = sb("m_tho", [128, D], BF16); R_tho = P.reg()
        ogT = sb("m_ogT", [128, 8, 128], BF16); R_ogT = P.reg()
        abc2 = [sb(f"m_abc{i}", [128, H, 128], F32) for i in range(2)]; R_abc2 = P.regs(2); abclk2 = [fw.dma_clock(f"abc{i}") for i in range(2)]

        def load_abc(blk):
            if blk < NB:
                k = blk % 2
                fw.dma(fw.sp, abclk2[k], abc2[k][:], a_dram[:, blk * 128:(blk + 1) * 128].partition_broadcast(128), reads=[R_cols], writes=[R_abc2[k]])
        _pT = sb("m_pT", [128, 128], F32); pT = [_pT, _pT]; _r = P.reg(); R_pT = [_r, _r]
        _sT = sb("m_sT", [128, 128], BF16); sT = [_sT, _sT]; _r = P.reg(); R_sT = [_r, _r]
        _qc = sb("m_qc", [128, DH], F32); qc = [_qc, _qc]; _r = P.reg(); R_qc = [_r, _r]
        sm = sb("m_sm", [128, H, 2], F32); den = sb("m_den", [128, H], F32); R_sm = P.regs(H); R_den = P.reg()
        R_sc = [R_pSC, R_pSC]; R_smc = [R_pSC] * H; R_SV = R_pSQ; R_QC = R_pSQ
        num = sb("m_num", [128, H, DH], F32); R_num = P.regs(H)
        _kw = sb("m_kw", [128, 2, 128], BF16); kw = [_kw, _kw]; _r = P.reg(); R_kw = [_r, _r]
        hst = sb("m_hst", [128, H, 6], F32); hmv = sb("m_hmv", [128, H, 2], F32); hsc = sb("m_hsc", [128, H], F32); R_hn = P.reg()

        nbg = GRP // 128

        def group_stage(g):
            tok0 = g * GRP
            rx = [P.R_xT[nbg * g + j] for j in range(nbg)]
            def conv_a1(ch):
                k = ch % 2
                pre, R_pre, acc, R_acc = pre2[k], R_pre2[k], acc2[k], R_acc2[k]
                p, Rp = fm()
                mm_group(P, p[:, 0:GRP], [(Win[:, dc, ch * 128:(ch + 1) * 128], P.xT[:, dc, tok0:tok0 + GRP]) for dc in range(8)], rx + [R_w, R_win], [Rp])
                fw.op(fw.act, A.copy, reads=[Rp], writes=[R_pre], out=pre[:, 3:3 + GRP], in_=p[:, 0:GRP])
                fw.op(fw.act, A.activation, reads=[Rp, R_w], writes=[R_acc], out=acc[:], in_=p[:, 0:GRP], func=AF.Identity, scale=cwh[:, ch, 3:4], bias=cbh[:, ch:ch + 1])
                fw.op(fw.pool, G_.tensor_copy, reads=[R_halo[ch]], writes=[R_pre], out=pre[:, 0:3], in_=halo[:, ch, :])

            def conv_a2(ch):
                k = ch % 2
                pre, R_pre, acc, R_acc = pre2[k], R_pre2[k], acc2[k], R_acc2[k]
                for tap in (2, 1, 0):
                    fw.op(fw.dve, V.scalar_tensor_tensor, reads=[R_pre, R_w, R_acc], writes=[R_acc], out=acc[:], in0=pre[:, tap:tap + GRP],
                          scalar=cwh[:, ch, tap:tap + 1], in1=acc[:], op0=ALU.mult, op1=ALU.add)
                fw.op(fw.pool, G_.tensor_copy, reads=[R_pre], writes=[R_halo[ch]], out=halo[:, ch, :], in_=pre[:, GRP:GRP + 3])
                fw.op(fw.act, A.activation, reads=[R_acc], writes=[R_qk[ch]], out=qk[:, ch, :], in_=acc[:], func=AF.Silu)

            def conv_b(ch):
                pass

            for ch in range(18):
                if ch >= 2:
                    conv_b(ch - 2)
                if ch < 16:
                    conv_a1(ch)
                if 1 <= ch < 17:
                    conv_a2(ch - 1)

        def front(blk):
            if True:
                g, j = divmod(blk, nbg)
                if j == 0:
                    group_stage(g)
                tok0 = g * GRP
                ogb, R_ogb = og[blk % 2], R_og[blk % 2]
                ts = slice(tok0 + j * 128, tok0 + (j + 1) * 128)
                ls = slice(j * 128, (j + 1) * 128)
                rxb = [P.R_xT[blk], R_w, R_win]
                abc, R_abc = abc2[blk % 2], R_abc2[blk % 2]
                load_abc(blk + 1)
                for half in range(2):
                    mm_group(P, pTM[:, half * 512:(half + 1) * 512],
                             [(P.xT[:, dc, ts], Win[:, dc, 2048 + half * 512:2048 + (half + 1) * 512]) for dc in range(8)], rxb, [R_pTM])
                fw.op(fw.act, A.copy, reads=[R_pTM], writes=[R_v], out=v_sb[:], in_=pTM[:])
                fw.op(fw.pool, G_.tensor_tensor, reads=[R_abc, R_w], writes=[R_abc], out=abc[:], in0=abc[:], in1=tri[:].unsqueeze(1).to_broadcast([128, H, 128]),
                      op=ALU.add)

        def front_B(blk, part=None):
            if True:
                g, j = divmod(blk, nbg)
                tok0 = g * GRP
                ogb, R_ogb = og[blk % 2], R_og[blk % 2]
                abc, R_abc = abc2[blk % 2], R_abc2[blk % 2]
                ts = slice(tok0 + j * 128, tok0 + (j + 1) * 128)
                ls = slice(j * 128, (j + 1) * 128)
                rxb = [P.R_xT[blk], R_w, R_win]

                def hv(h):
                    return (slice(h * DH, (h + 1) * DH), [qk[:, 2 * h + c, ls] for c in range(2)], [qk[:, 8 + 2 * h + c, ls] for c in range(2)],
                            [R_qk[2 * h], R_qk[2 * h + 1]], [R_qk[8 + 2 * h], R_qk[8 + 2 * h + 1]])

                def S1(h):
                    hs_, qch, kch, Rq, Rk = hv(h)
                    k = h % 2
                    sc = pSC[:, 128 * k:128 * (k + 1)]
                    fw.op(fw.act, A.activation, reads=[R_abc, R_cols], writes=[R_pT[k]], out=pT[k][:], in_=abc[:, h, :], func=AF.Exp, bias=cols[:, blk, h:h + 1])
                    mm_group(P, sc, [(kch[c], qch[c]) for c in range(2)], Rq + Rk, [R_sc[k]])
                    fw.op(fw.dve, V.scalar_tensor_tensor, reads=[R_sc[k], R_pT[k]], writes=[R_sT[k]], out=sT[k][:], in0=sc, scalar=1.0 / 16.0, in1=pT[k][:],
                          op0=ALU.mult, op1=ALU.mult)
                    for c in range(2):
                        fw.op(fw.pe, nc.tensor.transpose, reads=Rk + [P.R_const], writes=[P.R_pTR], out=P.pTR[:, c, :], in_=kch[c], identity=P.identb[:])
                    fw.op(fw.act, A.activation, reads=[P.R_pTR, R_cols], writes=[R_kw[k]], out=kw[k][:], in_=P.pTR[:, 0:2, :], func=AF.Identity,
                          scale=cols[:, blk, 12 + h:13 + h])

                def S2(h):
                    hs_, qch, kch, Rq, Rk = hv(h)
                    k = h % 2
                    c0 = 256 + 4 * h
                    mm_group(P, pSC[:, c0:c0 + 1], [(sT[k][:], onesb[:, 0:1])], [R_sT[k], R_w], [R_smc[h]])
                    mm_group(P, pSC[:, c0 + 1:c0 + 2], [(qch[c], n16[:, h, c:c + 1]) for c in range(2)], Rq + [R_C16[h]], [R_smc[h]])
                    mm_group(P, pSQ[:, 0:256], [(sT[k][:], v_sb[:, hs_])], [R_sT[k], R_v], [R_SV])
                    mm_group(P, pSQ[:, 256:512], [(qch[c], C16[:, h, c, :]) for c in range(2)], Rq + [R_C16[h]], [R_QC])
                    fw.op(fw.act, A.activation, reads=[R_QC, R_cols], writes=[R_qc[k]], out=qc[k][:], in_=pSQ[:, 256:512], func=AF.Identity,
                          scale=cols[:, blk, 4 + h:5 + h])
                    fw.op(fw.act, A.copy, reads=[R_smc[h]], writes=[R_sm[h]], out=sm[:, h, :], in_=pSC[:, c0:c0 + 2])
                    fw.op(fw.dve, V.tensor_tensor, reads=[R_SV, R_qc[k]], writes=[R_num[h]], out=num[:, h, :], in0=pSQ[:, 0:256], in1=qc[k][:], op=ALU.add)
                    fw.op(fw.dve, V.bn_stats, reads=[R_num[h]], writes=[R_hn], out=hst[:, h, :], in_=num[:, h, :])
                    for c in range(2):
                        mm_group(P, pCU[:, c, :], [(kw[k][:, c, :], v_sb[:, hs_])], [R_kw[k], R_v], [R_pCU])
                        mm_group(P, pSC[:, c0 + 2 + c:c0 + 3 + c], [(kw[k][:, c, :], onesb[:, 0:1])], [R_kw[k], R_w], [R_smc[h]])
                    wo = woldbc[:, h, blk:blk + 1]
                    fw.op(fw.dve, V.scalar_tensor_tensor, reads=[R_C32[h], R_wold, R_pCU], writes=[R_C32[h]], out=C32[:, h].rearrange("p c d -> p (c d)"),
                          in0=C32[:, h].rearrange("p c d -> p (c d)"), scalar=wo, in1=pCU[:].rearrange("p c d -> p (c d)"), op0=ALU.mult, op1=ALU.add)
                    fw.op(fw.dve, V.scalar_tensor_tensor, reads=[R_C32[h], R_wold, R_smc[h]], writes=[R_C32[h]], out=n32[:, h, :], in0=n32[:, h, :], scalar=wo,
                          in1=pSC[:, c0 + 2:c0 + 4], op0=ALU.mult, op1=ALU.add)
                    fw.op(fw.pool, G_.tensor_copy, reads=[R_C32[h]], writes=[R_C16[h]], out=C16[:, h].rearrange("p c d -> p (c d)"), in_=C32[:, h].rearrange("p c d -> p (c d)"))
                    fw.op(fw.pool, G_.tensor_copy, reads=[R_C32[h]], writes=[R_C16[h]], out=n16[:, h, :], in_=n32[:, h, :])

                if HEAD_SKEW:
                    for r in range(H + 1):
                        if r < H:
                            S1(r)
                        if r >= 1:
                            S2(r - 1)
                elif part == 0:
                    S1(0)
                    return
                else:
                    for r in range(H):
                        if not (part == 1 and r == 0):
                            S1(r)
                        S2(r)
                for half in range(2):
                    mm_group(P, pTM[:, half * 512:(half + 1) * 512],
                             [(P.xT[:, dc, ts], Win[:, dc, 3072 + half * 512:3072 + (half + 1) * 512]) for dc in range(8)], rxb, [R_pTM])
                fw.op(fw.act, A.activation, reads=[R_pTM], writes=[R_tho], out=tho[:], in_=pTM[:], func=AF.Tanh, scale=0.5)
                fw.op(fw.dve, V.tensor_tensor, reads=R_sm + [R_cols], writes=[R_den], out=den[:], in0=sm[:, :, 1], in1=cols[:, blk, 4:8], op=ALU.mult)
                fw.op(fw.dve, V.tensor_tensor, reads=R_sm + [R_den], writes=[R_den], out=den[:], in0=den[:], in1=sm[:, :, 0], op=ALU.add)
                fw.op(fw.dve, V.scalar_tensor_tensor, reads=[R_den], writes=[R_den], out=den[:], in0=den[:], scalar=-1.0, in1=den[:], op0=ALU.mult, op1=ALU.max)
                fw.op(fw.dve, V.tensor_tensor, reads=[R_den, R_cols], writes=[R_den], out=den[:], in0=den[:], in1=cols[:, blk, 8:12], op=ALU.max)
                fw.op(fw.dve, V.reciprocal, reads=[R_den], writes=[R_den], out=den[:], in_=den[:])
                for h in range(H):
                    fw.op(fw.dve, V.bn_aggr, reads=[R_hn], writes=[R_hn], out=hmv[:, h, :], in_=hst[:, h, :])
                fw.op(fw.dve, V.tensor_tensor, reads=[R_hn, R_den], writes=[R_hn], out=hsc[:], in0=hmv[:, :, 1], in1=den[:], op=ALU.mult)
                fw.op(fw.dve, V.tensor_tensor, reads=[R_hn, R_den], writes=[R_hn], out=hsc[:], in0=hsc[:], in1=den[:], op=ALU.mult)
                fw.op(fw.dve, V.tensor_scalar_add, reads=[R_hn], writes=[R_hn], out=hsc[:], in0=hsc[:], scalar1=LN_EPS)
                fw.op(fw.pool, G_.tensor_tensor, reads=[R_hn, P.R_const], writes=[R_hn], out=hsc[:], in0=hsc[:], in1=P.mhalf[:].to_broadcast([128, H]), op=ALU.pow)
                fw.op(fw.dve, V.tensor_tensor, reads=[R_hn, R_den], writes=[R_hn], out=hsc[:], in0=hsc[:], in1=den[:], op=ALU.mult)
                for h in range(H):
                    fw.op(fw.dve, V.tensor_scalar, reads=[R_num[h], R_hn], writes=[R_num[h]], out=num[:, h, :], in0=num[:, h, :], scalar1=hmv[:, h, 0:1],
                          scalar2=hsc[:, h:h + 1], op0=ALU.subtract, op1=ALU.mult)
                fw.op(fw.dve, V.scalar_tensor_tensor, reads=[R_tho] + R_num, writes=[R_ogb], out=ogb[:], in0=tho[:], scalar=1.0,
                      in1=num[:].rearrange("p h d -> p (h d)"), op0=ALU.add, op1=ALU.mult)

        def tailA(blk):
            if True:
                ogb, R_ogb = og[blk % 2], R_og[blk % 2]
                for c in range(8):
                    fw.op(fw.pe, nc.tensor.transpose, reads=[R_ogb, P.R_const], writes=[P.R_pTR], out=P.pTR[:, c, :], in_=ogb[:, c * 128:(c + 1) * 128],
                          identity=P.identb[:])
                fw.op(fw.act, A.copy, reads=[P.R_pTR], writes=[R_ogT], out=ogT[:], in_=P.pTR[:])
                for half in range(2):
                    mm_group(P, pTM[:, half * 512:(half + 1) * 512],
                             [(ogT[:, vc, :], Wout[:, vc, half * 512:(half + 1) * 512]) for vc in range(8)], [R_ogT, R_w], [R_pTM])
                residual_ln_out(P, tiles, pTM[:], R_pTM, x_src, dst_h, blk, ln_idx, lng, lnb, R_lnp, defer_cast=True)

        load_abc(0)
        for b in range(NB + 1):
            if b < NB:
                front(b)
            if 0 <= b - 1 < NB:
                tailA(b - 1)
            if b < NB:
                front_B(b)
            if 0 <= b - 1 < NB:
                zb_cast(P, z, R_z, b - 1)
                xT_finish(P, b - 1)
        P.phase_barrier()


S_FULL = 4096
T_MOE = 2048
_IN_SHAPES = {
    "gla_w_in": [1, D, 3088], "gla_w_gate_up": [1, 16, 512], "gla_norm_g": [1, D], "gla_w_out": [1, D, D],
    "mlstm_w_in": [1, D, 4104], "mlstm_w_out": [1, D, D],
    "router_w": [D, NE], "moe_w1": [2, NE, D, DFF], "moe_w3": [2, NE, D, DFF], "moe_w2": [2, NE, DFF, D],
    "ln_g": [2, 2, D], "ln_b": [2, 2, D],
    "l_gla_b_gate": [128, 4], "l_mb": [4, 2], "l_conv_w": [128, 16, 4], "l_conv_b": [128, 16], "l_mnormg": [128, 8],
    "c_tri": [128, 128], "c_trineg": [128, 128], "c_rmask": [128, 512], "c_sel": [4, 4, 128],
}
_NTL = S_FULL // TSL + 3
_SP_SHAPES = {
    "c_upper": ([128, 128], BF16), "c_ones16": ([128, 128], BF16), "c_thrp": ([128, 4, S_FULL // TSL], F32), "c_thrt": ([128, _NTL, 3], F32),
    "c_tokid": ([128, S_FULL // 128], I32), "c_oob": ([128, _NTL * TSL // 128], I32), "c_wb": ([128, 4, 8], F32), "c_w2b": ([128, 4, 4], F32),
}


def build_program(S=S_FULL, T=T_MOE):
    P = Prog(S)
    nc, fw = P.nc, P.fw
    x = P.din("x", [S, D])
    out = P.dout("out", [S, D])
    for k, (shp, dt_) in _SP_SHAPES.items():
        P.din(k, shp, dt_)
    for k, shp in _IN_SHAPES.items():
        if k not in ("ln_g", "ln_b"):
            P.din(k, shp)
    P.din("ln_g", [2, 2, D])
    P.din("ln_b", [2, 2, D])
    hA = P.dscr("hA", [S + 1, D])
    setup_common(P)
    P.bchk = nc.gpsimd.alloc_register("bchk")
    nc.gpsimd.reg_mov(P.bchk, S - 1)
    with ExitStack() as st0:
        zrow = P.sb("zrow", [1, D], F32, st0)
        Rz0 = P.reg()
        fw.op(fw.pool, nc.gpsimd.memset, writes=[Rz0], ap=zrow[:], constant=0.0)
        fw.dma(fw.sp, fw.dma_clock("zrow"), hA[S:S + 1, :], zrow[:], reads=[Rz0])
        P.phase_barrier()
    gla_phase(P, x, hA, 0, p0_src=x)
    moe_sparse_phase(P, 0, hA, hA, True)
    mlstm_phase(P, hA, hA, 2)
    moe_sparse_phase(P, 1, hA, out, False)
    fw.wait_all(fw.sp, list(fw._dmaclk.values()))
    fw.wait_all(fw.pool, list(fw._dmaclk.values()))
    return P


def host_consts(inputs):
    f32 = np.float32
    sel = np.zeros((4, 4, 128), f32)
    for h in range(4):
        sel[h, h, :] = 1.0
    rmask = np.ones((128, 512), f32)
    rmask[:, ::128] = 0.0
    c = {
        "c_identb": np.eye(128).astype(ml_dtypes.bfloat16),
        "c_identf": np.eye(128, dtype=f32),
        "c_tri": (np.arange(128)[:, None] <= np.arange(128)[None, :]).astype(f32),
        "c_rmask": rmask,
        "c_trineg": np.where(np.arange(128)[:, None] <= np.arange(128)[None, :], 0.0, -1e30).astype(f32),
        "c_sel": sel,
        "c_upper": (np.arange(128)[:, None] < np.arange(128)[None, :]).astype(ml_dtypes.bfloat16),
        "c_ones16": np.ones((128, 128), ml_dtypes.bfloat16),
        "c_thrp": np.broadcast_to((float(TSL) * np.arange(S_FULL // TSL))[None, None, :], (128, 4, S_FULL // TSL)).astype(f32).copy(),
        "c_thrt": np.broadcast_to((float(TSL) * np.arange(_NTL))[None, :, None], (128, _NTL, 3)).astype(f32).copy(),
        "c_tokid": (np.arange(S_FULL // 128)[None, :] * 128 + np.arange(128)[:, None]).astype(np.int32),
        "c_oob": np.full((128, _NTL * TSL // 128), S_FULL, np.int32),
        "c_wb": (np.arange(4)[None, :, None] * 1024 + np.arange(8)[None, None, :] * 128 + np.arange(128)[:, None, None]).astype(f32),
        "c_w2b": (np.arange(4)[None, :, None] * 512 + np.arange(4)[None, None, :] * 128 + np.arange(128)[:, None, None]).astype(f32),
        "l_gla_b_gate": np.ascontiguousarray(np.asarray(inputs["gla_b_gate"], f32)[0].reshape(4, 128).T),
        "l_mb": np.ascontiguousarray(np.asarray(inputs["mlstm_b_gates"], f32)[0].reshape(2, 4).T),
        "l_conv_w": np.ascontiguousarray(np.asarray(inputs["mlstm_conv_w"], f32)[0].reshape(4, 16, 128).transpose(2, 1, 0)),
        "l_conv_b": np.ascontiguousarray(np.asarray(inputs["mlstm_conv_b"], f32)[0].reshape(16, 128).T),
        "l_mnormg": np.ascontiguousarray(np.asarray(inputs["mlstm_norm_g"], f32)[0].reshape(8, 128).T),
    }
    return c


_PROG_CACHE = {}


def kernel(**inputs):
    x = np.asarray(inputs["x"], np.float32)
    B = x.shape[0]
    if "p" not in _PROG_CACHE:
        _PROG_CACHE["p"] = build_program()
    P = _PROG_CACHE["p"]
    shared = {k: np.ascontiguousarray(np.asarray(inputs[k], np.float32)) for k in
              ["gla_w_in", "gla_w_gate_up", "gla_norm_g", "gla_w_out", "mlstm_w_in", "mlstm_w_out", "router_w", "moe_w1", "moe_w3", "moe_w2", "ln_g", "ln_b"]}
    shared.update(host_consts(inputs))
    in_maps = [dict(shared, x=np.ascontiguousarray(x[b])) for b in range(B)]
    res = run_bass_kernel_spmd(P.nc, in_maps, core_ids=list(range(B)))
    return np.stack([np.asarray(r["out"], np.float32)[:S_FULL] for r in res.results], axis=0)


def _idma(fw, clk, out, in_, out_offset=None, in_offset=None, reads=(), writes=(), bounds_check=None):
    if not clk.name.endswith("_sw"):
        clk = fw.dma_clock(clk.name[2:] + "_sw")
    fw._deps(fw.pool, reads, writes, skip_clock=clk)
    kw = {}
    if bounds_check is not None:
        kw = dict(bounds_check=bounds_check, oob_is_err=False)
    ins = fw.nc.gpsimd.indirect_dma_start(out=out, out_offset=out_offset, in_=in_, in_offset=in_offset, **kw)
    ins.then_inc(clk.sem, 16)
    clk.count += 16
    for r in reads:
        r.r[clk] = clk.count
    for w in writes:
        w.w = (clk, clk.count)
        w.r = {}
    fw.n_inst += 1
    return ins


def moe_sparse_phase(P, layer, src_h, dst_h, make_xT):
    nc, fw = P.nc, P.fw
    S, NB = P.S, P.NB
    NC = NB
    KP = S // TSL
    NTL = KP + 3
    NSL = NTL * TSL
    NJ = TSL // 128
    V, A, G_ = nc.vector, nc.scalar, nc.gpsimd
    OFF = IndirectOffsetOnAxis
    w1t = P.dram["moe_w1"].rearrange("l e d f -> (l e d) f")
    w3t = P.dram["moe_w3"].rearrange("l e d f -> (l e d) f")
    w2t = P.dram["moe_w2"].rearrange("l e f d -> (l e f) d")
    slot_tok = P.dscr(f"slot_tok{layer}", [NSL, 1], I32)
    g4_dram = P.dscr(f"g4_dram{layer}", [S + 1, 4])
    with ExitStack() as st:
        sb = lambda n, s, d: P.sb(n, s, d, st)
        widx = sb("s_widx", [128, NTL, 4, 8], I32)
        w2idx = sb("s_w2idx", [128, NTL, 4, 4], I32)
        R_idx = P.reg("widx")
        lng = sb("s_lng", [128, D], F32); lnb = sb("s_lnb", [128, D], F32); R_lnp = P.reg()
        ck = fw.dma_clock("s_const")
        fw.dma(fw.sp, ck, lng[:], P.dram["ln_g"][layer, 1, :].partition_broadcast(128), writes=[R_lnp])
        fw.dma(fw.sp, ck, lnb[:], P.dram["ln_b"][layer, 1, :].partition_broadcast(128), writes=[R_lnp])
        pA = [P.ps(f"s_pA{i}", [128, 512], F32, st) for i in range(2)]
        pB = [P.ps(f"s_pB{i}", [128, 512], F32, st) for i in range(2)]
        pY = [P.ps(f"s_pY{i}", [128, 512], F32, st) for i in range(2)]
        R_pA, R_pB, R_pY, R_pR = P.regs(2), P.regs(2), P.regs(2), P.reg()
        W1 = [sb(f"s_W1_{s}", [128, 8, DFF], BF16) for s in range(2)]
        W3 = [sb(f"s_W3_{s}", [128, 8, DFF], BF16) for s in range(2)]
        W2 = [sb(f"s_W2_{s}", [128, 4, D], BF16) for s in range(2)]
        R_W1, R_W3, R_W2 = [P.regs(8), P.regs(8)], [P.regs(8), P.regs(8)], [P.regs(4), P.regs(4)]
        wclk = [[fw.dma_clock(f"s_w{m}_{s}") for s in range(2)] for m in range(3)]

        def load_w(s, jj, slot, part=None):
            jobs = [(0, dc) for dc in range(8)] + [(1, dc) for dc in range(8)] + [(2, fc) for fc in range(4)]
            if part is not None:
                jobs = jobs[:10] if part == 0 else jobs[10:]
            for m, c in jobs:
                if m == 0:
                    _idma(fw, wclk[0][slot], W1[slot][:, c, :], w1t, in_offset=OFF(ap=widx[:, s, jj, c:c + 1], axis=0), reads=[R_idx], writes=[R_W1[slot][c]])
                elif m == 1:
                    _idma(fw, wclk[1][slot], W3[slot][:, c, :], w3t, in_offset=OFF(ap=widx[:, s, jj, c:c + 1], axis=0), reads=[R_idx], writes=[R_W3[slot][c]])
                else:
                    _idma(fw, wclk[2][slot], W2[slot][:, c, :], w2t, in_offset=OFF(ap=w2idx[:, s, jj, c:c + 1], axis=0), reads=[R_idx], writes=[R_W2[slot][c]])

        with ExitStack() as st2:
            sb2 = lambda n, s, d: P.sb(n, s, d, st2)
            pR = P.ps("s_pR", [128, 512], F32, st2)
            wr = sb2("s_wr", [128, 8, NE], BF16); R_wr = P.reg()
            fw.dma(fw.pool, fw.dma_clock("s_wr"), wr[:], P.dram["router_w"].rearrange("(c p) e -> p c e", p=128), writes=[R_wr])
            aff = sb2("s_aff", [128, NC, NE], F32); am = sb2("s_am", [128, NC, NE], F32)
            k1 = sb2("s_k1", [128, NC, NE], F32); k2 = sb2("s_k2", [128, NC, NE], F32)
            t6 = sb2("s_t6", [128, NC, 4, 6], F32); gs = sb2("s_gs", [128, NC, 4], F32)
            gmax = sb2("s_gmax", [128, NC], F32); gmask = sb2("s_gmask", [128, NC, 4], F32)
            m1 = sb2("s_m1", [128, NC], F32); m2 = sb2("s_m2", [128, NC], F32)
            g4 = sb2("s_g4", [128, NC, 4], F32)
            gm16 = sb2("s_gm16", [128, NC * 4], BF16)
            upper = sb2("s_upper", [128, 128], BF16); ones16 = sb2("s_ones16", [128, 128], BF16)
            ones32 = sb2("s_ones32", [128, NC], F32)
            tot = sb2("s_tot", [128, 4, NC], F32); incl = sb2("s_incl", [128, 4, NC], F32)
            pw = sb2("s_pw", [128, NC, 4], F32)
            thrp = sb2("s_thrp", [128, 4, KP], F32); thrt = sb2("s_thrt", [128, NTL, 3], F32)
            cmpp = sb2("s_cmpp", [128, 4, KP], F32); cmpt = sb2("s_cmpt", [128, NTL, 3], F32)
            padg = sb2("s_padg", [128, 4], F32); cum = sb2("s_cum", [128, 4], F32); base = sb2("s_base", [128, 4], F32)
            gid = sb2("s_gid", [128, NTL], F32)
            posf = sb2("s_posf", [128, NC], F32); posi = sb2("s_posi", [128, NC], I32)
            tokid = sb2("s_tokid", [128, NC], I32); oobt = sb2("s_oob", [128, NSL // 128], I32)
            wb = sb2("s_wb", [128, 4, 8], F32); w2b = sb2("s_w2b", [128, 4, 4], F32)
            wf = sb2("s_wf", [128, NTL, 4, 8], F32); w2f = sb2("s_w2f", [128, NTL, 4, 4], F32)
            R_rt = P.reg("srt")
            for t_, name in [(upper, "c_upper"), (ones16, "c_ones16"), (thrp, "c_thrp"), (thrt, "c_thrt"), (tokid, "c_tokid"), (oobt, "c_oob"),
                             (wb, "c_wb"), (w2b, "c_w2b")]:
                fw.dma(fw.sp, ck, t_[:], P.dram[name], writes=[R_rt])
            fw.op(fw.pool, G_.memset, writes=[R_rt], ap=ones32[:], constant=1.0)
            sclk = fw.dma_clock("s_slot")
            fw.dma(fw.sp, sclk, slot_tok.rearrange("(p j) o -> p (j o)", p=128), oobt[:], reads=[R_rt])
            pRv = pR[:, 0:NC * NE].rearrange("p (c e) -> p c e", e=NE)
            for c in range(NC):
                mm_group(P, pRv[:, c, :], [(P.xT[:, dc, c * 128:(c + 1) * 128], wr[:, dc, :]) for dc in range(8)], [P.R_xT[c], R_wr], [R_pR])
            rt = dict(reads=[R_rt], writes=[R_rt])
            fw.op(fw.act, A.activation, reads=[R_pR], writes=[R_rt], out=aff[:], in_=pRv, func=AF.Sigmoid)
            a4 = aff[:].rearrange("p c (g k) -> p c g k", k=4)
            fw.op(fw.dve, V.tensor_tensor, **rt, out=t6[:, :, :, 0:2], in0=a4[:, :, :, 0:2], in1=a4[:, :, :, 2:4], op=ALU.add)
            fw.op(fw.dve, V.tensor_tensor, **rt, out=t6[:, :, :, 2:5], in0=a4[:, :, :, 0:3], in1=a4[:, :, :, 1:4], op=ALU.add)
            fw.op(fw.dve, V.tensor_tensor, **rt, out=t6[:, :, :, 5:6], in0=a4[:, :, :, 0:1], in1=a4[:, :, :, 3:4], op=ALU.add)
            fw.op(fw.dve, V.tensor_reduce, **rt, out=gs[:], in_=t6[:], axis=AX.X, op=ALU.max)
            fw.op(fw.dve, V.tensor_reduce, **rt, out=gmax[:], in_=gs[:], axis=AX.X, op=ALU.max)
            fw.op(fw.dve, V.tensor_tensor, **rt, out=gmask[:], in0=gs[:], in1=gmax[:].unsqueeze(2).to_broadcast([128, NC, 4]), op=ALU.is_equal)
            gm_b = gmask[:].unsqueeze(3).to_broadcast([128, NC, 4, 4])
            am4 = am[:].rearrange("p c (g k) -> p c g k", k=4)
            fw.op(fw.dve, V.tensor_tensor, **rt, out=am4, in0=a4, in1=gm_b, op=ALU.mult)
            fw.op(fw.dve, V.tensor_tensor, **rt, out=am4, in0=am4, in1=gm_b, op=ALU.add)
            fw.op(fw.dve, V.tensor_scalar_add, **rt, out=am[:], in0=am[:], scalar1=-1.0)
            fw.op(fw.dve, V.tensor_reduce, **rt, out=m1[:], in_=am[:], axis=AX.X, op=ALU.max)
            bc16 = lambda t_: t_[:].unsqueeze(2).to_broadcast([128, NC, NE])
            fw.op(fw.dve, V.tensor_tensor, **rt, out=k1[:], in0=am[:], in1=bc16(m1), op=ALU.is_equal)
            fw.op(fw.dve, V.scalar_tensor_tensor, **rt, out=am[:], in0=k1[:], scalar=-2.0, in1=am[:], op0=ALU.mult, op1=ALU.add)
            fw.op(fw.dve, V.tensor_reduce, **rt, out=m2[:], in_=am[:], axis=AX.X, op=ALU.max)
            fw.op(fw.dve, V.tensor_tensor, **rt, out=k2[:], in0=am[:], in1=bc16(m2), op=ALU.is_equal)
            fw.op(fw.dve, V.tensor_tensor, **rt, out=k1[:], in0=k1[:], in1=bc16(m1), op=ALU.mult)
            fw.op(fw.dve, V.tensor_tensor, **rt, out=k2[:], in0=k2[:], in1=bc16(m2), op=ALU.mult)
            fw.op(fw.dve, V.tensor_tensor, **rt, out=k1[:], in0=k1[:], in1=k2[:], op=ALU.add)
            fw.op(fw.dve, V.tensor_tensor, **rt, out=m1[:], in0=m1[:], in1=m2[:], op=ALU.add)
            fw.op(fw.dve, V.reciprocal, **rt, out=m1[:], in_=m1[:])
            fw.op(fw.dve, V.tensor_tensor, **rt, out=k1[:], in0=k1[:], in1=bc16(m1), op=ALU.mult)
            R_g4 = P.reg("g4")
            fw.op(fw.dve, V.tensor_reduce, reads=[R_rt], writes=[R_g4], out=g4[:], in_=k1[:].rearrange("p c (g j) -> p c j g", j=4), axis=AX.X, op=ALU.add)
            gclk = fw.dma_clock("s_g4")
            fw.dma(fw.sp, gclk, g4_dram[0:S, :].rearrange("(c p) j -> p c j", p=128), g4[:], reads=[R_g4])
            fw.dma(fw.sp, gclk, g4_dram[S:S + 1, :], lng[0:1, 0:4], reads=[R_lnp])
            fw.op(fw.dve, V.tensor_copy, **rt, out=gm16[:], in_=gmask[:].rearrange("p c g -> p (c g)"))
            mm_group(P, pR[:, 0:NC * 4], [(upper[:], gm16[:])], [R_rt], [R_pR])
            fw.op(fw.dve, V.tensor_copy, reads=[R_pR], writes=[R_rt], out=pw[:], in_=pR[:, 0:NC * 4].rearrange("p (c g) -> p c g", g=4))
            mm_group(P, pR[:, 0:NC * 4], [(ones16[:], gm16[:])], [R_rt], [R_pR])
            fw.op(fw.dve, V.tensor_copy, reads=[R_pR], writes=[R_rt], out=tot[:], in_=pR[:, 0:NC * 4].rearrange("p (c g) -> p g c", g=4))
            for g in range(4):
                fw.op(fw.dve, V.tensor_tensor_scan, **rt, out=incl[:, g, :], data0=ones32[:], data1=tot[:, g, :], initial=0.0, op0=ALU.mult, op1=ALU.add)
            ng = incl[:, :, NC - 1]
            fw.op(fw.dve, V.tensor_tensor, **rt, out=tot[:], in0=incl[:], in1=tot[:], op=ALU.subtract)
            fw.op(fw.dve, V.tensor_tensor, **rt, out=cmpp[:], in0=ng.unsqueeze(2).to_broadcast([128, 4, KP]), in1=thrp[:], op=ALU.is_gt)
            fw.op(fw.dve, V.tensor_reduce, **rt, out=padg[:], in_=cmpp[:], axis=AX.X, op=ALU.add)
            fw.op(fw.dve, V.tensor_scalar_mul, **rt, out=padg[:], in0=padg[:], scalar1=float(TSL))
            fw.op(fw.dve, V.tensor_tensor_scan, **rt, out=cum[:], data0=ones32[:, 0:4], data1=padg[:], initial=0.0, op0=ALU.mult, op1=ALU.add)
            fw.op(fw.dve, V.tensor_tensor, **rt, out=base[:], in0=cum[:], in1=padg[:], op=ALU.subtract)
            fw.op(fw.dve, V.tensor_tensor, **rt, out=pw[:], in0=pw[:], in1=tot[:].rearrange("p g c -> p c g"), op=ALU.add)
            fw.op(fw.dve, V.tensor_tensor, **rt, out=pw[:], in0=pw[:], in1=base[:].unsqueeze(1).to_broadcast([128, NC, 4]), op=ALU.add)
            fw.op(fw.dve, V.tensor_tensor, **rt, out=pw[:], in0=pw[:], in1=gmask[:], op=ALU.mult)
            fw.op(fw.dve, V.tensor_reduce, **rt, out=posf[:], in_=pw[:], axis=AX.X, op=ALU.add)
            fw.op(fw.dve, V.tensor_copy, **rt, out=posi[:], in_=posf[:])
            fw.op(fw.dve, V.tensor_tensor, **rt, out=cmpt[:], in0=cum[:, 0:3].unsqueeze(1).to_broadcast([128, NTL, 3]), in1=thrt[:], op=ALU.is_le)
            fw.op(fw.dve, V.tensor_reduce, **rt, out=gid[:], in_=cmpt[:], axis=AX.X, op=ALU.add)
            for s in range(NTL):
                fw.op(fw.dve, V.scalar_tensor_tensor, **rt, out=wf[:, s], in0=gid[:, s:s + 1].unsqueeze(2).to_broadcast([128, 4, 8]), scalar=4096.0, in1=wb[:],
                      op0=ALU.mult, op1=ALU.add)
                fw.op(fw.dve, V.scalar_tensor_tensor, **rt, out=w2f[:, s], in0=gid[:, s:s + 1].unsqueeze(2).to_broadcast([128, 4, 4]), scalar=2048.0, in1=w2b[:],
                      op0=ALU.mult, op1=ALU.add)
            if layer:
                fw.op(fw.dve, V.tensor_scalar_add, **rt, out=wf[:], in0=wf[:], scalar1=float(layer * NE * D))
                fw.op(fw.dve, V.tensor_scalar_add, **rt, out=w2f[:], in0=w2f[:], scalar1=float(layer * NE * DFF))
            fw.op(fw.dve, V.tensor_copy, reads=[R_rt], writes=[R_idx], out=widx[:], in_=wf[:])
            fw.op(fw.dve, V.tensor_copy, reads=[R_rt], writes=[R_idx], out=w2idx[:], in_=w2f[:])
            R_slot = P.reg("slot")
            fw.wait_all(fw.pool, [sclk])
            for c in range(NC):
                _idma(fw, sclk, slot_tok, tokid[:, c:c + 1], out_offset=OFF(ap=posi[:, c:c + 1], axis=0), reads=[R_rt], writes=[R_slot])
            P.phase_barrier()
        zb2 = [P.zb[0], sb("s_zb1", [128, D], BF16)]; R_zb2 = [P.R_zb[0], P.reg()]
        pTR2 = [P.pTR, P.ps("s_pTR1", [128, 8, 128], BF16, st)]; R_pTR2 = [P.R_pTR, P.reg()]
        hrow = [sb(f"s_hrow{i}", [128, NJ, D], F32) for i in range(2)]; R_hrow = [P.regs(NJ), P.regs(NJ)]
        g4t = [sb(f"s_g4t{i}", [128, NJ, 4], F32) for i in range(2)]; R_g4t = [P.regs(NJ), P.regs(NJ)]
        idx = [sb(f"s_idx{i}", [128, NJ], I32) for i in range(2)]; R_ix = P.regs(2)
        iclk = [fw.dma_clock(f"s_ix{i}") for i in range(2)]
        hclk = [fw.dma_clock(f"s_h{i}") for i in range(2)]
        oclk = [fw.dma_clock(f"s_o{i}") for i in range(2)]
        hs = [sb(f"s_hs{i}", [128, 512], F32) for i in range(2)]; R_hs = P.regs(2)
        hp = [sb(f"s_hp{i}", [128, 4, 512], BF16) for i in range(2)]
        R_hp = [[P.reg() for f in range(4)] for i in range(2)]
        lnT = {"junk": sb("s_junk", [128, D], BF16), "R_junk": P.reg(), "s": [sb(f"s_lns{i}", [128, 8], F32) for i in range(2)], "R": P.regs(2), "ctr": [0]}
        xTt = [P.xT[:, :, i * TSL:(i + 1) * TSL] for i in range(2)]
        R_xt = [P.regs(NJ), P.regs(NJ)]

        def load_tile(s):
            b = s % 2
            fw.dma(fw.sp, iclk[b], idx[b][:], slot_tok[s * TSL:(s + 1) * TSL, :].rearrange("(p j) o -> p (j o)", j=NJ), writes=[R_ix[b]])
            for j in range(NJ):
                _idma(fw, hclk[b], hrow[b][:, j, :], src_h, in_offset=OFF(ap=idx[b][:, j:j + 1], axis=0), reads=[R_ix[b]], writes=[R_hrow[b][j]])
                _idma(fw, hclk[b], g4t[b][:, j, :], g4_dram, in_offset=OFF(ap=idx[b][:, j:j + 1], axis=0), reads=[R_ix[b]], writes=[R_g4t[b][j]])

        def prep_tile(s):
            b = s % 2
            for j in range(NJ):
                q = j % 2
                fw.op(fw.act, A.copy, reads=[R_hrow[b][j]], writes=[R_zb2[q]], out=zb2[q][:], in_=hrow[b][:, j, :])
                for c in range(8):
                    fw.op(fw.pe, nc.tensor.transpose, reads=[R_zb2[q], P.R_const], writes=[R_pTR2[q]], out=pTR2[q][:, c, :],
                          in_=zb2[q][:, c * 128:(c + 1) * 128], identity=P.identb[:])
                fw.op(fw.dve, V.tensor_copy, reads=[R_pTR2[q]], writes=[R_xt[b][j]], out=xTt[b][:, :, j * 128:(j + 1) * 128], in_=pTR2[q][:])
                fw.op(fw.act, A.mul, reads=[R_hrow[b][j]], writes=[R_hrow[b][j]], out=hrow[b][:, j, :], in_=hrow[b][:, j, :], mul=ALPHA)

        ycnt = [0]

        def H(b, t5, slot, hb):
            rx = [R_xt[b][t5 * 4 + j] for j in range(4)]
            xs = lambda dc: xTt[b][:, dc, t5 * 512:(t5 + 1) * 512]
            for fc in range(4):
                a_, b_ = pA[fc % 2], pB[fc % 2]
                mm_group(P, a_[:], [(W1[slot][:, dc, fc * 128:(fc + 1) * 128], xs(dc)) for dc in range(8)], rx + R_W1[slot], [R_pA[fc % 2]])
                mm_group(P, b_[:], [(W3[slot][:, dc, fc * 128:(fc + 1) * 128], xs(dc)) for dc in range(8)], rx + R_W3[slot], [R_pB[fc % 2]])
                fw.op(fw.act, A.activation, reads=[R_pA[fc % 2]], writes=[R_hs[fc % 2]], out=hs[fc % 2][:], in_=a_[:], func=AF.Silu)
                fw.op(fw.dve, V.tensor_tensor, reads=[R_hs[fc % 2], R_pB[fc % 2]], writes=[R_hp[hb][fc]], out=hp[hb][:, fc, :], in0=hs[fc % 2][:], in1=b_[:],
                      op=ALU.mult)

        def Y(b, t5, slot, hb, jj):
            for tc in range(4):
                j = t5 * 4 + tc
                for dh in range(2):
                    k = ycnt[0] % 2
                    ycnt[0] += 1
                    mm_group(P, pY[k][:], [(hp[hb][:, fc, tc * 128:(tc + 1) * 128], W2[slot][:, fc, dh * 512:(dh + 1) * 512]) for fc in range(4)],
                             R_hp[hb] + R_W2[slot], [R_pY[k]])
                    ysl = hrow[b][:, j, dh * 512:(dh + 1) * 512]
                    fw.op(fw.dve, V.scalar_tensor_tensor, reads=[R_pY[k], R_g4t[b][j], R_hrow[b][j]], writes=[R_hrow[b][j]], out=ysl, in0=pY[k][:],
                          scalar=g4t[b][:, j, jj:jj + 1], in1=ysl, op0=ALU.mult, op1=ALU.add)

        def finish_sub(s, j):
            b = s % 2
            z = hrow[b][:, j, :]
            _ln_two_regs(P, z, [R_hrow[b][j]], lng, lnb, R_lnp, on_dve=True)
            _idma(fw, oclk[b], dst_h[0:S, :], z, out_offset=OFF(ap=idx[b][:, j:j + 1], axis=0), reads=[R_hrow[b][j], R_ix[b]], bounds_check=P.bchk)

        wpend = []
        pending = []

        def drain(n):
            for _ in range(n):
                if not pending:
                    return
                s_, j_ = pending.pop(0)
                finish_sub(s_, j_)
                if j_ == NJ - 1 and s_ + 2 < NTL:
                    load_tile(s_ + 2)

        units = [(s, jj) for s in range(NTL) for jj in range(4)]
        load_tile(0)
        load_w(0, 0, 0)
        prep_tile(0)
        load_w(0, 1, 1)
        load_tile(1)
        prev = None
        hb = 0
        for u, (s, jj) in enumerate(units):
            b, slot = s % 2, u % 2
            if jj == 3 and s + 1 < NTL:
                prep_tile(s + 1)
            for t5 in range(NJ // 4):
                H(b, t5, slot, hb)
                if prev is not None:
                    Y(*prev[:5])
                    if prev[5] and units[prev[6]][1] == 3:
                        pending.extend((units[prev[6]][0], j) for j in range(NJ))
                    if prev[5]:
                        pu = prev[6]
                        if pu + 2 < len(units):
                            load_w(units[pu + 2][0], units[pu + 2][1], pu % 2)
                    drain(2)
                last = (t5 == NJ // 4 - 1)
                prev = (b, t5, slot, hb, jj, last, u)
                hb ^= 1
        Y(*prev[:5])
        pending.extend((NTL - 1, j) for j in range(NJ))
        drain(len(pending))
        P.phase_barrier()
    if make_xT:
        with ExitStack() as st:
            NXB = 6
            xin = [P.sb(f"s_xin{i}", [128, D], F32, st) for i in range(NXB)]
            R_xin = [[P.reg()] for i in range(NXB)]
            clk = [fw.dma_clock(f"s_xin{i}") for i in range(NXB)]
            for bb in range(NB):
                k = bb % NXB
                fw.dma(fw.sp, clk[k], xin[k][:], dst_h[bb * 128:(bb + 1) * 128, :], writes=R_xin[k])
                _to_xT_two(P, xin[k][:], R_xin[k], bb)
            P.phase_barrier()
```
